# Optimizing a Trainium2 kernel written in Bass

```python
import math
import jax, jax.numpy as jnp
from jax import lax
import numpy as np


D_MODEL = 2048
BATCH = 2
SEQ = 4096
DEPTH = 1

ATTN_WIDTH = D_MODEL // 2
ATTN_HEAD_DIM = 128
N_ATTN_HEADS = ATTN_WIDTH // ATTN_HEAD_DIM
SSM_WIDTH = D_MODEL - ATTN_WIDTH
SSM_HEAD_DIM = 64
N_SSM_HEADS = SSM_WIDTH // SSM_HEAD_DIM
N_SSM_GROUPS = 2
SSM_HEADS_PER_GROUP = N_SSM_HEADS // N_SSM_GROUPS
D_STATE = 128
SSM_CONV = 4
SSM_CHUNK = 256
SSM_CONV_CH = SSM_WIDTH + 2 * N_SSM_GROUPS * D_STATE
MOBA_BLOCK = 256
MOBA_TOPK = 3
MOBA_Q_CHUNK = 32
REL_BUCKETS = 32
REL_MAX_DIST = 128
FFN_DIM = 5632
FFN_CONV = 3
IN_COLS = 3 * ATTN_WIDTH + 2 * SSM_WIDTH + 2 * N_SSM_GROUPS * D_STATE + N_SSM_HEADS
EPS = 1e-6

kernel_name = 'hymba_moba_ssd_convffn_adaln'


def rms_norm(x, g):
    xf = x.astype(jnp.float32)
    y = xf * lax.rsqrt(jnp.mean(xf * xf, axis=-1, keepdims=True) + EPS)
    return (y * g.astype(jnp.float32)).astype(x.dtype)


def causal_dwconv(x, w, b):
    k = w.shape[0]
    y = lax.conv_general_dilated(x, w[:, None, :].astype(x.dtype), window_strides=(1,),
                                 padding=[(k - 1, 0)], dimension_numbers=('NWC', 'WIO', 'NWC'),
                                 feature_group_count=x.shape[-1])
    return y + b.astype(x.dtype)


def pad_seq(x, mult):
    p = (-x.shape[1]) % mult
    return jnp.pad(x, [(0, 0), (0, p)] + [(0, 0)] * (x.ndim - 2))


def rel_bucket(dist):
    n = jnp.maximum(dist, 0)
    max_exact = REL_BUCKETS // 2
    nf = jnp.maximum(n, max_exact).astype(jnp.float32)
    large = max_exact + (jnp.log(nf / max_exact) / math.log(REL_MAX_DIST / max_exact)
                         * (REL_BUCKETS - max_exact)).astype(jnp.int32)
    large = jnp.minimum(large, REL_BUCKETS - 1)
    return jnp.where(n < max_exact, n, large)


def gather_blocks(blocks, sel):
    return jax.vmap(jax.vmap(lambda kb, s: kb[s]))(blocks, sel)


def moba_attention(q, k, v, rel_bias):
    bsz, s = q.shape[:2]
    q, k, v = [pad_seq(t, MOBA_BLOCK).transpose(0, 2, 1, 3) for t in (q, k, v)]
    sp = q.shape[2]
    nb = sp // MOBA_BLOCK
    n_sel = min(MOBA_TOPK, nb - 1)
    k_blocks = k.reshape(bsz, N_ATTN_HEADS, nb, MOBA_BLOCK, ATTN_HEAD_DIM)
    v_blocks = v.reshape(bsz, N_ATTN_HEADS, nb, MOBA_BLOCK, ATTN_HEAD_DIM)
    k_mean = jnp.mean(k_blocks, axis=3)
    scale = ATTN_HEAD_DIM ** -0.5
    bias_t = rel_bias.T.astype(jnp.float32)
    head_idx = jnp.arange(N_ATTN_HEADS)[:, None, None, None]

    def chunk(ci):
        q0 = ci * MOBA_Q_CHUNK
        blk = q0 // MOBA_BLOCK
        qc = lax.dynamic_slice_in_dim(q, q0, MOBA_Q_CHUNK, axis=2)
        q_pos = q0 + jnp.arange(MOBA_Q_CHUNK)
        k_own = lax.dynamic_index_in_dim(k_blocks, blk, axis=2, keepdims=False)
        v_own = lax.dynamic_index_in_dim(v_blocks, blk, axis=2, keepdims=False)
        dist_own = q_pos[:, None] - (blk * MOBA_BLOCK + jnp.arange(MOBA_BLOCK))[None, :]
        logit_own = (jnp.einsum('bhqd,bhkd->bhqk', qc, k_own).astype(jnp.float32) * scale
                     + bias_t[:, rel_bucket(dist_own)])
        logit_own = jnp.where(dist_own >= 0, logit_own, -jnp.inf)
        if n_sel == 0:
            p_own = jax.nn.softmax(logit_own, axis=-1).astype(v.dtype)
            return jnp.einsum('bhqk,bhkd->bhqd', p_own, v_own)
        gate = jnp.einsum('bhqd,bhnd->bhqn', qc, k_mean).astype(jnp.float32)
        gate = jnp.where(jnp.arange(nb) < blk, gate, -jnp.inf)
        _, sel = lax.top_k(gate, n_sel)
        k_sel = gather_blocks(k_blocks, sel)
        v_sel = gather_blocks(v_blocks, sel)
        sel_pos = sel[..., None] * MOBA_BLOCK + jnp.arange(MOBA_BLOCK)
        dist_sel = q_pos[:, None, None] - sel_pos
        logit_sel = (jnp.einsum('bhqd,bhqtkd->bhqtk', qc, k_sel).astype(jnp.float32) * scale
                     + bias_t[head_idx, rel_bucket(dist_sel)])
        slot_ok = (jnp.arange(n_sel) < blk)[:, None]
        logit_sel = jnp.where(slot_ok, logit_sel, -jnp.inf)
        logits = jnp.concatenate(
            [logit_sel.reshape(bsz, N_ATTN_HEADS, MOBA_Q_CHUNK, n_sel * MOBA_BLOCK), logit_own], axis=-1)
        p = jax.nn.softmax(logits, axis=-1).astype(v.dtype)
        p_sel = p[..., :n_sel * MOBA_BLOCK].reshape(bsz, N_ATTN_HEADS, MOBA_Q_CHUNK, n_sel, MOBA_BLOCK)
        p_own = p[..., n_sel * MOBA_BLOCK:]
        return (jnp.einsum('bhqtk,bhqtkd->bhqd', p_sel, v_sel)
                + jnp.einsum('bhqk,bhkd->bhqd', p_own, v_own))

    out = lax.map(chunk, jnp.arange(sp // MOBA_Q_CHUNK))
    out = out.transpose(1, 0, 3, 2, 4).reshape(bsz, sp, N_ATTN_HEADS * ATTN_HEAD_DIM)
    return out[:, :s]


def segsum(x):
    t = x.shape[-1]
    xe = jnp.broadcast_to(x[..., :, None], x.shape + (t,))
    xe = jnp.where(jnp.tril(jnp.ones((t, t), bool), -1), xe, 0.0)
    xs = jnp.cumsum(xe, axis=-2)
    return jnp.where(jnp.tril(jnp.ones((t, t), bool)), xs, -jnp.inf)


def ssd_mixer(xs, z, bmat, cmat, dt_raw, conv_w, conv_b, dt_bias, a_log, d_skip, norm_g):
    bsz, s = xs.shape[:2]
    gn = N_SSM_GROUPS * D_STATE
    xbc = jax.nn.silu(causal_dwconv(jnp.concatenate([xs, bmat, cmat], axis=-1), conv_w, conv_b))
    x_h = xbc[..., :SSM_WIDTH].reshape(bsz, s, N_SSM_HEADS, SSM_HEAD_DIM).astype(jnp.float32)
    bm = xbc[..., SSM_WIDTH:SSM_WIDTH + gn].astype(jnp.float32)
    cm = xbc[..., SSM_WIDTH + gn:].astype(jnp.float32)
    dt = jax.nn.softplus(dt_raw.astype(jnp.float32) + dt_bias.astype(jnp.float32))
    a = -jnp.exp(a_log.astype(jnp.float32))
    xdt = pad_seq(x_h * dt[..., None], SSM_CHUNK)
    adt = pad_seq(dt * a, SSM_CHUNK)
    bm, cm = pad_seq(bm, SSM_CHUNK), pad_seq(cm, SSM_CHUNK)
    sp = xdt.shape[1]
    nc = sp // SSM_CHUNK
    X = xdt.reshape(bsz, nc, SSM_CHUNK, N_SSM_GROUPS, SSM_HEADS_PER_GROUP, SSM_HEAD_DIM)
    A = adt.reshape(bsz, nc, SSM_CHUNK, N_SSM_GROUPS, SSM_HEADS_PER_GROUP).transpose(0, 3, 4, 1, 2)
    Bm = bm.reshape(bsz, nc, SSM_CHUNK, N_SSM_GROUPS, D_STATE)
    Cm = cm.reshape(bsz, nc, SSM_CHUNK, N_SSM_GROUPS, D_STATE)
    A_cs = jnp.cumsum(A, axis=-1)
    Lm = jnp.exp(segsum(A))
    y_diag = jnp.einsum('bclgn,bcsgn,bgrcls,bcsgrp->bclgrp', Cm, Bm, Lm, X)
    decay_states = jnp.exp(A_cs[..., -1:] - A_cs)
    states = jnp.einsum('bclgn,bgrcl,bclgrp->bcgrpn', Bm, decay_states, X)
    states = jnp.concatenate([jnp.zeros_like(states[:, :1]), states], axis=1)
    chunk_tot = jnp.pad(A_cs[..., -1], ((0, 0), (0, 0), (0, 0), (1, 0)))
    decay_chunk = jnp.exp(segsum(chunk_tot))
    new_states = jnp.einsum('bgrzc,bcgrpn->bzgrpn', decay_chunk, states)
    prev_states = new_states[:, :-1]
    y_off = jnp.einsum('bclgn,bcgrpn,bgrcl->bclgrp', Cm, prev_states, jnp.exp(A_cs))
    y = (y_diag + y_off).reshape(bsz, sp, N_SSM_HEADS, SSM_HEAD_DIM)[:, :s]
    y = y + d_skip.astype(jnp.float32)[:, None] * x_h
    y = y.reshape(bsz, s, SSM_WIDTH) * jax.nn.silu(z.astype(jnp.float32))
    yg = y.reshape(bsz, s, N_SSM_GROUPS, SSM_WIDTH // N_SSM_GROUPS)
    yg = yg * lax.rsqrt(jnp.mean(yg * yg, axis=-1, keepdims=True) + EPS)
    return (yg.reshape(bsz, s, SSM_WIDTH) * norm_g.astype(jnp.float32)).astype(xs.dtype)


def conv_ffn(h, w_up, conv_w, conv_b, w_down):
    u = causal_dwconv(h @ w_up, conv_w, conv_b)
    g, val = jnp.split(u, 2, axis=-1)
    return (jax.nn.silu(g) * val) @ w_down


def setup_inputs(seed: int = 0) -> dict:
    key = jax.random.key(seed)
    ks = jax.random.split(key, 24)
    f32 = jnp.float32
    nrm = lambda k, shape, sc: jax.random.normal(k, shape, f32) * sc
    dt0 = jnp.exp(jax.random.uniform(ks[10], (DEPTH, N_SSM_HEADS), f32)
                  * (math.log(0.1) - math.log(0.001)) + math.log(0.001))
    return {
        'x': nrm(ks[0], (BATCH, SEQ, D_MODEL), 1.0),
        'c': nrm(ks[1], (BATCH, D_MODEL), 1.0),
        'w_ada': nrm(ks[2], (DEPTH, D_MODEL, 6 * D_MODEL), 0.5 * D_MODEL ** -0.5),
        'b_ada': nrm(ks[3], (DEPTH, 6 * D_MODEL), 0.01),
        'norm_mix_g': 1.0 + nrm(ks[4], (DEPTH, D_MODEL), 0.05),
        'w_in': nrm(ks[5], (DEPTH, D_MODEL, IN_COLS), D_MODEL ** -0.5),
        'rel_bias': nrm(ks[6], (REL_BUCKETS, N_ATTN_HEADS), 0.2),
        'attn_norm_g': 1.0 + nrm(ks[7], (DEPTH, ATTN_WIDTH), 0.05),
        'conv_ssm_w': nrm(ks[8], (DEPTH, SSM_CONV, SSM_CONV_CH), SSM_CONV ** -0.5),
        'conv_ssm_b': nrm(ks[9], (DEPTH, SSM_CONV_CH), 0.01),
        'dt_bias': dt0 + jnp.log(-jnp.expm1(-dt0)),
        'a_log': jnp.log(jax.random.uniform(ks[11], (DEPTH, N_SSM_HEADS), f32, 1.0, 16.0)),
        'd_skip': 1.0 + nrm(ks[12], (DEPTH, N_SSM_HEADS), 0.1),
        'ssm_norm_g': 1.0 + nrm(ks[13], (DEPTH, SSM_WIDTH), 0.05),
        'w_out': nrm(ks[14], (DEPTH, ATTN_WIDTH + SSM_WIDTH, D_MODEL), (ATTN_WIDTH + SSM_WIDTH) ** -0.5),
        'norm_ffn_g': 1.0 + nrm(ks[15], (DEPTH, D_MODEL), 0.05),
        'w_up': nrm(ks[16], (DEPTH, D_MODEL, 2 * FFN_DIM), D_MODEL ** -0.5),
        'conv_ffn_w': nrm(ks[17], (DEPTH, FFN_CONV, 2 * FFN_DIM), FFN_CONV ** -0.5),
        'conv_ffn_b': nrm(ks[18], (DEPTH, 2 * FFN_DIM), 0.01),
        'w_down': nrm(ks[19], (DEPTH, FFN_DIM, D_MODEL), FFN_DIM ** -0.5),
        'final_norm_g': 1.0 + nrm(ks[20], (D_MODEL,), 0.05),
    }


def reference(x, c, w_ada, b_ada, norm_mix_g, w_in, rel_bias, attn_norm_g, conv_ssm_w, conv_ssm_b,
              dt_bias, a_log, d_skip, ssm_norm_g, w_out, norm_ffn_g, w_up, conv_ffn_w, conv_ffn_b,
              w_down, final_norm_g):
    bsz, s, _ = x.shape
    gn = N_SSM_GROUPS * D_STATE
    sizes = [ATTN_WIDTH, ATTN_WIDTH, ATTN_WIDTH, SSM_WIDTH, SSM_WIDTH, gn, gn, N_SSM_HEADS]
    split_at = [int(v) for v in np.cumsum(sizes)[:-1]]
    for l in range(DEPTH):
        mod = jax.nn.silu(c) @ w_ada[l] + b_ada[l]
        shift_m, scale_m, gate_m, shift_f, scale_f, gate_f = jnp.split(mod[:, None, :], 6, axis=-1)
        h = rms_norm(x, norm_mix_g[l]) * (1.0 + scale_m) + shift_m
        proj = h @ w_in[l]
        q, k, v, xs, z, bmat, cmat, dt_raw = jnp.split(proj, split_at, axis=-1)
        hd = (bsz, s, N_ATTN_HEADS, ATTN_HEAD_DIM)
        attn = moba_attention(q.reshape(hd), k.reshape(hd), v.reshape(hd), rel_bias)
        attn = rms_norm(attn, attn_norm_g[l])
        ssm = ssd_mixer(xs, z, bmat, cmat, dt_raw, conv_ssm_w[l], conv_ssm_b[l], dt_bias[l],
                        a_log[l], d_skip[l], ssm_norm_g[l])
        mix = jnp.concatenate([attn, ssm], axis=-1) @ w_out[l]
        x = x + gate_m * mix
        h = rms_norm(x, norm_ffn_g[l]) * (1.0 + scale_f) + shift_f
        x = x + gate_f * conv_ffn(h, w_up[l], conv_ffn_w[l], conv_ffn_b[l], w_down[l])
    return rms_norm(x, final_norm_g)
```

```python
import numpy as np
import concourse.bass as bass
import concourse.mybir as mybir
from concourse.bass_utils import run_bass_kernel_spmd

F32 = mybir.dt.float32
BF16 = mybir.dt.bfloat16
AF = mybir.ActivationFunctionType
ALU = mybir.AluOpType
AX = mybir.AxisListType

D = 2048
NT = 32
OWN0 = 23
NOWN = 9
FFN = 5632
NCT = 44
EPS = 1e-6
NEG = -30000.0
COL_Q, COL_K, COL_V, COL_XS, COL_Z, COL_B, COL_C, COL_DT = 0, 1024, 2048, 3072, 4096, 5120, 5376, 5632
IN_COLS = 5648
OFFS = [-384, -256, -128, 0, 128]
ARENA_WORDS = 53000
import os
KSTOP = int(os.environ.get('KSTOP', '99'))
KSUB = os.environ.get('KSUB', '')
ZERO_INIT = int(os.environ.get('ZERO_INIT', '1'))


class Buf:
    __slots__ = ("name", "w", "r", "dsem", "dcnt", "ex")

    def __init__(self, name, ex=False):
        self.name = name
        self.ex = ex
        self.w = None
        self.r = []
        self.dsem = None
        self.dcnt = 0


class Tracker:
    ENG = ("sync", "scalar", "vector", "gpsimd", "tensor")

    def __init__(self, nc):
        self.nc = nc
        self.streams = {n: [] for n in self.ENG}
        self.esem = {n: nc.alloc_semaphore("es_" + n) for n in self.ENG}
        self.ecnt = {n: 0 for n in self.ENG}
        self.waited = {n: {} for n in self.ENG}
        self.dbufs = []
        self.nsem = 5

    def _wait(self, eng, deps):
        need = {}
        for d in deps:
            if d is None:
                continue
            sem, val, src = d
            if src == eng and eng == "tensor":
                continue
            k = sem.num
            if need.get(k, (None, 0))[1] < val:
                need[k] = (sem, val)
        for k, (sem, val) in need.items():
            if self.waited[eng].get(k, 0) >= val:
                continue
            self.waited[eng][k] = val
            self.streams[eng].append(lambda e, sem=sem, val=val: e.wait_ge(sem, val))

    def op(self, eng, emit, r=(), w=(), sig=True):
        deps = []
        for b in r:
            deps.append(b.w)
            if b.ex:
                deps.extend(d for d in b.r if d[2] != eng)
        for b in w:
            deps.append(b.w)
            deps.extend(b.r)
        self._wait(eng, deps)
        sem = self.esem[eng]
        if sig:
            self.ecnt[eng] += 1
            rec = (sem, self.ecnt[eng], eng)
            self.streams[eng].append(lambda e, emit=emit, sem=sem: emit(e).then_inc(sem, 1))
        else:
            rec = (sem, self.ecnt[eng] + 1, eng)
            self.streams[eng].append(lambda e, emit=emit: emit(e))
        for b in r:
            b.r.append(rec)
        for b in w:
            b.w = rec
            b.r = []

    def dma(self, q, out, in_, sb, r=(), w=()):
        deps = []
        for b in r:
            deps.append(b.w)
        for b in w:
            deps.append(b.w)
            deps.extend(b.r)
        self._wait(q, deps)
        if sb.dsem is None:
            sb.dsem = self.nc.alloc_semaphore("ds_%d" % len(self.dbufs))
            self.dbufs.append(sb)
            self.nsem += 1
        sb.dcnt += 16
        rec = (sb.dsem, sb.dcnt, None)
        sem = sb.dsem
        self.streams[q].append(lambda e, out=out, in_=in_, sem=sem: e.dma_start(out=out, in_=in_).then_inc(sem, 16))
        for b in r:
            b.r.append(rec)
        for b in w:
            b.w = rec
            b.r = []

    def barrier(self):
        deps = [(self.esem[n], self.ecnt[n], n + "_x") for n in self.ENG if self.ecnt[n] > 0]
        deps += [(b.dsem, b.dcnt, None) for b in self.dbufs]
        for n in self.ENG:
            self._wait(n, [d for d in deps if d[2] != n + "_x"])


class Arena:
    def __init__(self, nc):
        self.t = nc.alloc_sbuf_tensor("arena", [128, ARENA_WORDS], F32)
        self.off = 0
        self.hi = 0

    def f32(self, n):
        a = self.t[:, self.off:self.off + n]
        self.off += n
        self.hi = max(self.hi, self.off)
        assert self.off <= ARENA_WORDS, ("arena overflow", self.off)
        return a

    def bf16(self, n):
        w = (n + 1) // 2
        a = self.t[:, self.off:self.off + w].bitcast(BF16)
        self.off += w
        self.hi = max(self.hi, self.off)
        assert self.off <= ARENA_WORDS, ("arena overflow", self.off)
        return a


def build_nc():
    nc = bass.Bass("TRN2", target_bir_lowering=False)

    def din(name, shape):
        return nc.dram_tensor(name, list(shape), F32, kind="ExternalInput").ap()

    xl = din("xl", [4096, D])
    smalls = din("smalls", [128, 560])
    consts = din("consts", [128, 512])
    eall_d = din("eall", [16, 2048])
    w_ada = din("w_ada", [D, 6 * D])
    b_ada = din("b_ada", [1, 6 * D])
    vecs = din("vecs", [3, D])
    w_in = din("w_in", [D, IN_COLS])
    btiles = din("btiles", [8, 5, 128, 512])
    w_out = din("w_out", [D, D])
    w_up = din("w_up", [D, 2 * FFN])
    w_down = din("w_down", [FFN, D])
    y_out = nc.dram_tensor("y", [1024, D], F32, kind="ExternalOutput").ap()
    modv = nc.dram_tensor("modv", [6, D], F32).ap()
    ymix = nc.dram_tensor("ymix", [NOWN * 128, D], F32).ap()
    xmid = nc.dram_tensor("xmid", [1024, D], F32).ap()
    ffsc = nc.dram_tensor("ffsc", [1024, D], F32).ap()

    T = Tracker(nc)
    A = Arena(nc)
    PS = [nc.alloc_psum_tensor("ps%d" % i, [128, 512], F32) for i in range(8)]
    PB = [Buf("ps%d" % i, ex=True) for i in range(8)]
    rr = {"s": 0, "m": 0}
    ACCP = [(PS[3], PB[3]), (PS[4], PB[4]), (PS[5], PB[5]), (PS[6], PB[6])]

    rr["n"] = 3

    def ps_s():
        i = rr["s"] % rr["n"]
        rr["s"] += 1
        return PS[i], PB[i]

    def ps_m():
        i = 7
        return PS[i], PB[i]

    class _Stop(Exception):
        pass

    def phase_done(idx):
        T.barrier()
        if idx == KSTOP:
            raise _Stop()

    def sub_done(tag):
        if tag == KSUB:
            T.barrier()
            raise _Stop()

    def body():
        zB = Buf("zero_init")
        third = ARENA_WORDS // 4
        if ZERO_INIT:
            T.op("vector", lambda e: e.memset(A.t[:, 0:2 * third], 0.0), w=[zB])
            T.op("gpsimd", lambda e: e.memset(A.t[:, 2 * third:3 * third], 0.0), w=[zB])
            T.op("scalar", lambda e: e.activation(out=A.t[:, 3 * third:ARENA_WORDS], in_=A.t[:, 0:ARENA_WORDS - 3 * third], func=AF.Copy), r=[zB], w=[zB])
            for i in range(8):
                T.op("vector", lambda e, i=i: e.memset(PS[i][:, :], 0.0), w=[PB[i]])
            T.barrier()
        cst = A.f32(512)
        cstB = Buf("cst")
        ident32, tri, umat, ones32 = cst[:, 0:128], cst[:, 128:256], cst[:, 256:384], cst[:, 384:512]
        sm = A.f32(560)
        smB = Buf("sm")
        vtile = sm[:, 0:32]
        validneg = sm[:, 32:48]
        c_arr = sm[:, 48:64]
        dtb = sm[:, 64:80]
        alog = sm[:, 80:96]
        dsk = sm[:, 96:112]
        rb31 = sm[:, 112:120]
        gcat = sm[:, 120:136]
        cw_ssm = sm[:, 136:184]
        cb_ssm = sm[:, 184:196]
        cwf = sm[:, 196:460]
        cbf = sm[:, 460:548]
        ident16 = A.bf16(128)
        eall16 = A.bf16(2048)
        misc = A.f32(128)
        miscB = Buf("misc")
        sc_silu = misc[:, 0:16]
        a_neg = misc[:, 16:32]
        mk5 = misc[:, 32:112]
        ssq_a = A.f32(NOWN * 8)
        ssq_s = A.f32(NOWN * 4)
        ssqB = Buf("ssq")
        junk = A.bf16(256)
        junkB = Buf("junk")
        stat = A.f32(16)
        statB = Buf("stat")

        T.dma("sync", cst, consts[:, :], cstB, w=[cstB])
        T.dma("sync", sm, smalls[:, :], smB, w=[smB])
        e32 = A.f32(2048)
        e32B = Buf("e32")
        T.dma("sync", e32[0:16, :], eall_d[:, :], e32B, w=[e32B])
        identB = Buf("ident16")
        T.op("vector", lambda e: e.tensor_copy(out=ident16, in_=ident32), r=[cstB], w=[identB])
        T.op("vector", lambda e: e.tensor_copy(out=eall16[0:16, :], in_=e32[0:16, :]), r=[e32B], w=[identB])
        T.op("scalar", lambda e: e.activation(out=sc_silu, in_=c_arr, func=AF.Silu), r=[smB], w=[miscB])
        T.op("scalar", lambda e: e.activation(out=a_neg, in_=alog, func=AF.Exp), r=[smB], w=[miscB])
        T.op("vector", lambda e: e.tensor_scalar(out=a_neg, in0=a_neg, scalar1=-1.0, scalar2=None, op0=ALU.mult), r=[miscB], w=[miscB])
        for i in range(5):
            blk = 11 + i
            T.op("vector", lambda e, i=i: e.tensor_copy(out=mk5[:, i * 16:(i + 1) * 16], in_=validneg), r=[smB], w=[miscB])
            T.op("vector", lambda e, i=i, blk=blk: e.memset(mk5[:, i * 16 + blk:(i + 1) * 16], -1e30), w=[miscB])
        T.op("vector", lambda e: e.memset(ssq_a, 0.0), w=[ssqB])
        T.op("vector", lambda e: e.memset(ssq_s, 0.0), w=[ssqB])
        A.off -= 2048
        base0 = A.off

        NWA = 6
        wA = [A.f32(2048) for _ in range(NWA)]
        wAB = [Buf("wA%d" % i) for i in range(NWA)]
        tmp0 = A.f32(2048)
        tmp0B = Buf("tmp0")
        T.barrier()
        w_ada_v = w_ada.rearrange("(p k) n -> p k n", k=16)
        cnt = 0
        for m in range(6):
            T.dma("sync", tmp0[0:1, :], b_ada[0:1, m * D:(m + 1) * D], tmp0B, w=[tmp0B])
            for k in range(16):
                sl = cnt % NWA
                q = "sync" if cnt % 2 == 0 else "scalar"
                cnt += 1
                T.dma(q, wA[sl], w_ada_v[:, k, m * D:(m + 1) * D], wAB[sl], w=[wAB[sl]])
                for cchunk in range(4):
                    aps, apb = ACCP[cchunk]
                    T.op("tensor", lambda e, aps=aps, sl=sl, k=k, cchunk=cchunk: e.matmul(aps[0:1, :], sc_silu[:, k:k + 1], wA[sl][:, cchunk * 512:(cchunk + 1) * 512], start=(k == 0), stop=(k == 15)),
                         r=[wAB[sl], miscB], w=[apb], sig=(cchunk == 3))
            for cchunk in range(4):
                aps, apb = ACCP[cchunk]
                T.op("vector", lambda e, aps=aps, cchunk=cchunk: e.tensor_tensor(out=tmp0[0:1, cchunk * 512:(cchunk + 1) * 512], in0=aps[0:1, :], in1=tmp0[0:1, cchunk * 512:(cchunk + 1) * 512], op=ALU.add),
                     r=[apb], w=[tmp0B])
            T.dma("scalar", modv[m:m + 1, :], tmp0[0:1, :], tmp0B, r=[tmp0B])
        phase_done(0)

        def bcast_row(dst, dstB, src_row):
            T.dma("sync", dst, src_row.partition_broadcast(128)[:, 0, :], dstB, w=[dstB])

        A.off = base0
        hT = A.bf16(16 * 4096)
        hTv = hT.rearrange("p (k t) -> p k t", k=16)
        hTB = [Buf("hT%d" % t) for t in range(NT)]
        baseB = A.off
        gs_m = A.f32(2048)
        sh_m = A.f32(2048)
        gsB = Buf("gs_m")
        xt = [A.f32(2048), A.f32(2048)]
        xtB = [Buf("xt0"), Buf("xt1")]
        junkbig = A.bf16(2048)
        h16 = [A.bf16(2048), A.bf16(2048)]
        h16B = [Buf("h16a"), Buf("h16b")]

        def load_gs(gs, gsB_, sh, g_row, m_shift, m_scale, tmpap, tmpB):
            bcast_row(tmpap, tmpB, vecs[g_row:g_row + 1, :])
            bcast_row(gs, gsB_, modv[m_scale:m_scale + 1, :])
            bcast_row(sh, gsB_, modv[m_shift:m_shift + 1, :])
            T.op("vector", lambda e: e.scalar_tensor_tensor(out=gs, in0=gs, scalar=1.0, in1=tmpap, op0=ALU.add, op1=ALU.mult),
                 r=[tmpB], w=[gsB_])

        load_gs(gs_m, gsB, sh_m, 0, 0, 1, xt[0], xtB[0])

        def norm_tile(x_ap, xB, gs, sh, gsB_, vcol, out16, out16B, st, junkbig):
            T.op("vector", lambda e: e.memset(st[:, 0:1], 0.0), w=[statB])
            T.op("scalar", lambda e: e.activation(out=junkbig, in_=x_ap, func=AF.Square, accum_out=st[:, 0:1]), r=[xB], w=[junkB, statB])
            T.op("vector", lambda e: e.tensor_scalar(out=st[:, 1:2], in0=st[:, 0:1], scalar1=1.0 / D, scalar2=EPS, op0=ALU.mult, op1=ALU.add), w=[statB])
            T.op("scalar", lambda e: e.activation(out=st[:, 1:2], in_=st[:, 1:2], func=AF.Sqrt), w=[statB])
            T.op("vector", lambda e: e.reciprocal(out=st[:, 1:2], in_=st[:, 1:2]), w=[statB])
            if vcol is not None:
                T.op("vector", lambda e: e.tensor_tensor(out=st[:, 1:2], in0=st[:, 1:2], in1=vcol, op=ALU.mult), r=[smB], w=[statB])
            T.op("vector", lambda e: e.scalar_tensor_tensor(out=x_ap, in0=x_ap, scalar=st[:, 1:2], in1=gs, op0=ALU.mult, op1=ALU.mult),
                 r=[gsB_, statB], w=[xB])
            if vcol is not None:
                T.op("vector", lambda e: e.scalar_tensor_tensor(out=out16, in0=sh, scalar=vcol, in1=x_ap, op0=ALU.mult, op1=ALU.add),
                     r=[gsB_, xB, smB], w=[out16B])
            else:
                T.op("vector", lambda e: e.tensor_tensor(out=out16, in0=sh, in1=x_ap, op=ALU.add), r=[gsB_, xB], w=[out16B])

        def transpose_to(src16, srcB, dstv, dstB, tok0, ntok=128):
            for half in range(2):
                ps, pb = ps_s()
                pv = ps[:, :].bitcast(BF16).rearrange("p (k t) -> p k t", k=8)
                for kk in range(8):
                    kc = half * 8 + kk
                    T.op("tensor", lambda e, pv=pv, kk=kk, kc=kc: e.transpose(pv[:, kk, :], src16[:, kc * 128:(kc + 1) * 128], ident16),
                         r=[srcB, identB], w=[pb], sig=(kk == 7))
                eng = "scalar" if half == 0 else "vector"
                if eng == "scalar":
                    T.op(eng, lambda e, pv=pv, half=half: e.activation(out=dstv[:, half * 8:(half + 1) * 8, tok0:tok0 + 128], in_=pv, func=AF.Copy), r=[pb], w=[dstB])
                else:
                    T.op(eng, lambda e, pv=pv, half=half: e.tensor_copy(out=dstv[:, half * 8:(half + 1) * 8, tok0:tok0 + 128], in_=pv), r=[pb], w=[dstB])

        xl_t = xl.rearrange("(t p) d -> t p d", p=128)
        T.dma("sync", xt[1], xl_t[0], xtB[1], w=[xtB[1]])
        for t in range(NT):
            sl = (t + 1) % 2
            if t + 1 < NT:
                T.dma("sync", xt[t % 2], xl_t[t + 1], xtB[t % 2], w=[xtB[t % 2]])
            norm_tile(xt[sl], xtB[sl], gs_m, sh_m, gsB, vtile[:, t:t + 1], h16[t % 2], h16B[t % 2], stat, junkbig)
            transpose_to(h16[t % 2], h16B[t % 2], hTv, hTB[t], t * 128)
        phase_done(1)

        A.off = baseB
        w_in_v = w_in.rearrange("(k p) n -> p k n", p=128)
        wst = A.f32(2048)
        wstB = Buf("wst")
        wq16, wk16, wv16 = A.bf16(2048), A.bf16(2048), A.bf16(2048)
        wqB, wkB, wvB = Buf("wq"), Buf("wk"), Buf("wv")
        KT = A.bf16(4096)
        KTB = Buf("KT")
        Vaug = A.bf16(NT * 130)
        Vv = Vaug.rearrange("p (t c) -> p t c", c=130)
        VB = Buf("V")
        q32 = A.f32(NOWN * 128)
        q16 = A.bf16(NOWN * 128)
        qB = Buf("q")
        negmT = A.bf16(NOWN * 128)
        negB = Buf("negmT")
        ksum = A.f32(16)
        ksB = Buf("ksum")
        PT = [A.bf16(512), A.bf16(512)]
        PTB = [Buf("PT0"), Buf("PT1")]
        ssb = [A.f32(512), A.f32(512)]
        ssbB = [Buf("ssb0"), Buf("ssb1")]
        biasT = A.f32(5 * 512)
        biasB = Buf("biasT")
        osb = [A.f32(128), A.f32(128)]
        osbB = [Buf("o0"), Buf("o1")]
        gw = A.f32(64)
        gwB = Buf("gw")
        negm = A.f32(16)
        endB1 = A.off
        T.op("vector", lambda e: e.memset(Vaug, 1.0), w=[VB])

        def load_w(dst16, dstB, col0, ncols=128, eng="vector"):
            T.dma("sync", wst.rearrange("p (k c) -> p k c", k=16)[:, :, 0:ncols], w_in_v[:, :, col0:col0 + ncols], wstB, w=[wstB])
            dv = dst16.rearrange("p (k c) -> p k c", k=16)[:, :, 0:ncols] if ncols != 128 else dst16.rearrange("p (k c) -> p k c", k=16)
            sv = wst.rearrange("p (k c) -> p k c", k=16)[:, :, 0:ncols]
            if eng == "vector":
                T.op("vector", lambda e: e.tensor_copy(out=dv, in_=sv), r=[wstB], w=[dstB])
            else:
                T.op("scalar", lambda e: e.activation(out=dv, in_=sv, func=AF.Copy), r=[wstB], w=[dstB])

        chunks = [(23, 1), (24, 4), (28, 4)]
        SCALE = 128 ** -0.5
        kslot = 0
        for h in range(8):
            load_w(wk16, wkB, COL_K + h * 128, eng="vector")
            load_w(wv16, wvB, COL_V + h * 128, eng="scalar")
            load_w(wq16, wqB, COL_Q + h * 128, eng="vector")
            T.dma("sync", biasT.rearrange("p (o q) -> p o q", o=5), btiles[h].rearrange("o k q -> k o q"), biasB, w=[biasB])
            wkv = wk16.rearrange("p (k c) -> p k c", k=16)
            wvv = wv16.rearrange("p (k c) -> p k c", k=16)
            wqv = wq16.rearrange("p (k c) -> p k c", k=16)
            sub_done("ld%d" % h)
            for tc in range(8):
                ps, pb = ps_s()
                for kc in range(16):
                    T.op("tensor", lambda e, ps=ps, kc=kc, tc=tc: e.matmul(ps[:, :], wkv[:, kc, :], hTv[:, kc, tc * 512:(tc + 1) * 512], start=(kc == 0), stop=(kc == 15)),
                         r=[wkB] + hTB[tc * 4:tc * 4 + 4], w=[pb], sig=(kc == 15))
                T.op("scalar", lambda e, ps=ps, tc=tc: e.activation(out=KT[:, tc * 512:(tc + 1) * 512], in_=ps[:, :], func=AF.Copy), r=[pb], w=[KTB])
                T.op("vector", lambda e, ps=ps, tc=tc: e.tensor_reduce(out=ksum[:, tc * 2:tc * 2 + 2], in_=ps[:, :].rearrange("p (b t) -> p b t", b=2), axis=AX.X, op=ALU.add),
                     r=[pb], w=[ksB])
            sub_done("k%d" % h)
            for tg in range(8):
                ps, pb = ps_s()
                for tt in range(4):
                    t = tg * 4 + tt
                    for kc in range(16):
                        T.op("tensor", lambda e, ps=ps, kc=kc, t=t, tt=tt: e.matmul(ps[:, tt * 128:(tt + 1) * 128], hTv[:, kc, t * 128:(t + 1) * 128], wvv[:, kc, :], start=(kc == 0), stop=(kc == 15)),
                             r=[wvB, hTB[t]], w=[pb], sig=(kc == 15 and tt == 3))
                T.op("vector", lambda e, ps=ps, tg=tg: e.tensor_copy(out=Vv[:, tg * 4:(tg + 1) * 4, 0:128], in_=ps[:, :].rearrange("p (t c) -> p t c", t=4)), r=[pb], w=[VB])
            sub_done("v%d" % h)
            for (qt0, nq) in chunks:
                ps, pb = ps_s()
                W = nq * 128
                c0 = (qt0 - OWN0) * 128
                for kc in range(16):
                    T.op("tensor", lambda e, ps=ps, kc=kc, qt0=qt0, W=W: e.matmul(ps[:, 0:W], wqv[:, kc, :], hTv[:, kc, qt0 * 128:qt0 * 128 + W], start=(kc == 0), stop=(kc == 15)),
                         r=[wqB] + hTB[qt0:qt0 + nq], w=[pb], sig=(kc == 15))
                T.op("scalar", lambda e, ps=ps, c0=c0, W=W: e.activation(out=q32[:, c0:c0 + W], in_=ps[:, 0:W], func=AF.Copy, scale=SCALE), r=[pb], w=[qB])
                T.op("vector", lambda e, c0=c0, W=W: e.tensor_copy(out=q16[:, c0:c0 + W], in_=q32[:, c0:c0 + W]), r=[qB], w=[qB])
            sub_done("proj%d" % h)
            for qi in range(NOWN):
                qt = OWN0 + qi
                blk = qt // 2
                mi = blk - 11
                ps, pb = ps_m()
                T.op("tensor", lambda e, ps=ps, qi=qi: e.matmul(ps[:, 0:16], q32[:, qi * 128:(qi + 1) * 128], ksum, start=True, stop=True), r=[qB, ksB], w=[pb])
                T.op("vector", lambda e, ps=ps, mi=mi: e.tensor_tensor(out=gw[:, 0:16], in0=ps[:, 0:16], in1=mk5[:, mi * 16:(mi + 1) * 16], op=ALU.add), r=[pb, miscB], w=[gwB])
                T.op("vector", lambda e: e.max(out=gw[:, 16:24], in_=gw[:, 0:16]), w=[gwB])
                T.op("vector", lambda e: e.tensor_scalar(out=gw[:, 24:25], in0=gw[:, 18:19], scalar1=-1e29, scalar2=None, op0=ALU.max), w=[gwB])
                T.op("vector", lambda e: e.tensor_scalar(out=gw[:, 32:48], in0=gw[:, 0:16], scalar1=gw[:, 24:25], scalar2=None, op0=ALU.is_ge), w=[gwB])
                T.op("vector", lambda e: e.tensor_scalar(out=negm, in0=gw[:, 32:48], scalar1=-1.0, scalar2=-NEG, op0=ALU.add, op1=ALU.mult), w=[gwB])
                T.op("vector", lambda e, blk=blk: e.memset(negm[:, blk:blk + 1], 0.0), w=[gwB])
                ps2, pb2 = ps_m()
                T.op("tensor", lambda e, ps2=ps2: e.transpose(ps2[0:16, 128:256], negm, ident32), r=[gwB, cstB], w=[pb2])
                T.op("vector", lambda e, ps2=ps2, qi=qi: e.tensor_copy(out=negmT[0:16, qi * 128:(qi + 1) * 128], in_=ps2[0:16, 128:256]), r=[pb2], w=[negB])
            sub_done("gate%d" % h)
            for (qt0, nq) in chunks:
                W = nq * 128
                c0 = (qt0 - OWN0) * 128
                lastblk = (qt0 + nq - 1) // 2
                nkt = 2 * lastblk + 2

                def emit_S(kt, c0=c0, W=W):
                    ps, pb = ps_s()
                    kb = kt // 2
                    T.op("tensor", lambda e, ps=ps, kt=kt: e.matmul(ps[:, 0:W], KT[:, kt * 128:(kt + 1) * 128], q16[:, c0:c0 + W], start=True, stop=False),
                         r=[KTB, qB], w=[pb], sig=False)
                    T.op("tensor", lambda e, ps=ps, kb=kb: e.matmul(ps[:, 0:W], eall16[0:16, kb * 128:(kb + 1) * 128], negmT[0:16, c0:c0 + W], start=False, stop=True),
                         r=[negB, identB], w=[pb])
                    return ps, pb

                def emit_E(kt, ps, pb, sl, W=W, qt0=qt0, h=h):
                    off = qt0 * 128 - kt * 128
                    if off <= 128:
                        oi = OFFS.index(off)
                        T.op("vector", lambda e: e.tensor_tensor(out=ssb[sl][:, 0:W], in0=ps[:, 0:W], in1=biasT[:, oi * 512:oi * 512 + W], op=ALU.add),
                             r=[pb, biasB], w=[ssbB[sl]])
                        T.op("scalar", lambda e: e.activation(out=PT[sl][:, 0:W], in_=ssb[sl][:, 0:W], func=AF.Exp), r=[ssbB[sl]], w=[PTB[sl]])
                    else:
                        T.op("scalar", lambda e: e.activation(out=PT[sl][:, 0:W], in_=ps[:, 0:W], func=AF.Exp, bias=rb31[:, h:h + 1]),
                             r=[pb, smB], w=[PTB[sl]])

                def emit_PV(kt, sl, nq=nq, qt0=qt0):
                    kb = kt // 2
                    for j in range(nq):
                        qblk = (qt0 + j) // 2
                        if kb > qblk:
                            continue
                        aps, apb = ACCP[j]
                        last = (kt == 2 * qblk + 1)
                        T.op("tensor", lambda e, aps=aps, j=j, last=last: e.matmul(aps[:, 0:129], PT[sl][:, j * 128:(j + 1) * 128], Vv[:, kt, 0:129], start=(kt == 0), stop=last),
                             r=[PTB[sl], VB], w=[apb], sig=last)

                cur = emit_S(0)
                for kt in range(nkt):
                    nxt = emit_S(kt + 1) if kt + 1 < nkt else None
                    sl = kslot % 2
                    kslot += 1
                    emit_E(kt, cur[0], cur[1], sl)
                    emit_PV(kt, sl)
                    cur = nxt
                sub_done("sc%d_%d" % (h, qt0))
                for j in range(nq):
                    qi = qt0 + j - OWN0
                    aps, apb = ACCP[j]
                    so = (qi + h) % 2
                    T.op("vector", lambda e, aps=aps: e.reciprocal(out=gw[:, 56:57], in_=aps[:, 128:129]), r=[apb], w=[gwB])
                    T.op("scalar", lambda e, aps=aps, so=so: e.activation(out=osb[so], in_=aps[:, 0:128], func=AF.Copy, scale=gw[:, 56:57]), r=[apb, gwB], w=[osbB[so]])
                    T.op("scalar", lambda e, so=so, qi=qi, h=h: e.activation(out=junk[:, 0:128], in_=osb[so], func=AF.Square, accum_out=ssq_a[:, qi * 8 + h:qi * 8 + h + 1]),
                         r=[osbB[so]], w=[junkB, ssqB])
                    T.dma("scalar", ymix[qi * 128:(qi + 1) * 128, h * 128:(h + 1) * 128], osb[so], osbB[so], r=[osbB[so]])
        phase_done(2)

        A.off = baseB
        wst = A.f32(2048)
        wstB = Buf("wst2")
        lhsD4s = [wst[:, 0:512], wst[:, 768:1280]]
        MT4s = [wst[:, 512:768].bitcast(BF16), wst[:, 1280:1536].bitcast(BF16)]
        LDBs = [Buf("lhsD4a"), Buf("lhsD4b")]
        MTBs = [Buf("MT4a"), Buf("MT4b")]
        wx16 = [A.bf16(2048), A.bf16(2048)]
        wb16, wc16 = A.bf16(2048), A.bf16(2048)
        wzd16 = A.bf16(16 * 260)
        wsB = Buf("w_ssd")
        ubuf = [A.f32(516) for _ in range(4)]
        ubB = [Buf("ub%d" % i) for i in range(4)]
        cv = A.f32(512)
        cvB = Buf("cv")
        xhT = A.f32(512)
        xhTB = Buf("xhT")
        BT16 = [A.bf16(512), A.bf16(512)]
        CT16 = [A.bf16(512), A.bf16(512)]
        BTB = [Buf("BT0"), Buf("BT1")]
        CTB = [Buf("CT0"), Buf("CT1")]
        xh16 = [A.bf16(1024), A.bf16(1024)]
        xhB = [Buf("xh0"), Buf("xh1")]
        Btok = [A.bf16(512), A.bf16(512)]
        BtokB = [Buf("Btok0"), Buf("Btok1")]
        zs = [A.bf16(1024), A.bf16(1024)]
        zsB = [Buf("zs0"), Buf("zs1")]
        dtt = [A.f32(16), A.f32(16)]
        dtB = [Buf("dt0"), Buf("dt1")]
        S32 = A.f32(256)
        S16all = A.bf16(1024)
        SB_ = Buf("S")
        S16B = Buf("S16")
        smv = A.f32(128)
        smvB = Buf("smv")
        Xd = A.bf16(1024)
        XdB = Buf("Xd")
        yoffs = [A.f32(256), A.f32(256)]
        yoffBs = [Buf("yoff0"), Buf("yoff1")]
        Gms = [A.f32(128), A.f32(128)]
        GmBs = [Buf("Gm0"), Buf("Gm1")]
        yg = [A.f32(256), A.f32(256)]
        ygB = [Buf("yg0"), Buf("yg1")]
        assert A.off <= ARENA_WORDS

        def load_w2(dst16, col0, ncols, c_off=0):
            wv_ = wst.rearrange("p (k c) -> p k c", k=16)[:, :, 0:ncols]
            T.dma("sync", wv_, w_in_v[:, :, col0:col0 + ncols], wstB, w=[wstB])
            dv = dst16.rearrange("p (k c) -> p k c", k=16)[:, :, c_off:c_off + ncols]
            T.op("vector", lambda e: e.tensor_copy(out=dv, in_=wv_), r=[wstB], w=[wsB])

        def b3(ap, n):
            return ap.unsqueeze(2).to_broadcast([128, n, 64])

        rr["n"] = 4
        for hg in range(4):
            g = hg // 2
            T.barrier()
            load_w2(wx16[0], COL_XS + hg * 256, 128)
            load_w2(wx16[1], COL_XS + hg * 256 + 128, 128)
            load_w2(wb16, COL_B + g * 128, 128)
            load_w2(wc16, COL_C + g * 128, 128)
            load_w2(wzd16, COL_Z + hg * 256, 128, c_off=0)
            load_w2(wzd16, COL_Z + hg * 256 + 128, 128, c_off=128)
            load_w2(wzd16, COL_DT + hg * 4, 4, c_off=256)
            T.barrier()
            wzdv = wzd16.rearrange("p (k c) -> p k c", k=16)
            T.op("vector", lambda e: e.memset(S32, 0.0), w=[SB_])
            for i in range(4):
                T.op("vector", lambda e, i=i: e.memset(ubuf[i], 0.0), w=[ubB[i]])
            ctile = [hg * 2, hg * 2 + 1, 8 + g, 10 + g]

            def early(tc, hg=hg, ctile=ctile, wzdv=wzdv):
                db = tc % 2
                own_chunk = (tc >= 5)
                steps = []
                srcs = [(wx16[0], 0), (wx16[1], 1), (wb16, 2)] + ([(wc16, 3)] if own_chunk else [])

                def MM():
                    for (w16, ui) in srcs:
                        wv_ = w16.rearrange("p (k c) -> p k c", k=16)
                        ps, pb = ps_s()
                        for kc in range(16):
                            T.op("tensor", lambda e, kc=kc, ps=ps, wv_=wv_: e.matmul(ps[:, :], wv_[:, kc, :], hTv[:, kc, tc * 512:(tc + 1) * 512], start=(kc == 0), stop=(kc == 15)),
                                 r=[wsB] + hTB[tc * 4:tc * 4 + 4], w=[pb], sig=(kc == 15))
                        ub = ubuf[ui]
                        T.op("vector", lambda e, ub=ub: e.tensor_copy(out=ub[:, 1:4], in_=ub[:, 513:516]), w=[ubB[ui]])
                        T.op("scalar", lambda e, ub=ub, ps=ps: e.activation(out=ub[:, 4:516], in_=ps[:, :], func=AF.Copy), r=[pb], w=[ubB[ui]])
                steps.append(MM)

                def CV(ui):
                    ub = ubuf[ui]
                    ct = ctile[ui]
                    T.op("vector", lambda e: e.tensor_scalar(out=cv, in0=ub[:, 4:516], scalar1=cw_ssm[:, ct * 4 + 3:ct * 4 + 4], scalar2=cb_ssm[:, ct:ct + 1], op0=ALU.mult, op1=ALU.add),
                         r=[ubB[ui], smB], w=[cvB])
                    for jj in range(1, 4):
                        T.op("vector", lambda e, jj=jj: e.scalar_tensor_tensor(out=cv, in0=ub[:, 4 - jj:516 - jj], scalar=cw_ssm[:, ct * 4 + 3 - jj:ct * 4 + 4 - jj], in1=cv, op0=ALU.mult, op1=ALU.add),
                             r=[ubB[ui], smB], w=[cvB])
                    if ui < 2:
                        T.op("scalar", lambda e: e.activation(out=xhT, in_=cv, func=AF.Silu), r=[cvB], w=[xhTB])
                        ps2, pb2 = ps_s()
                        for tt in range(4):
                            T.op("tensor", lambda e, tt=tt: e.transpose(ps2[:, tt * 128:(tt + 1) * 128], xhT[:, tt * 128:(tt + 1) * 128], ident32),
                                 r=[xhTB, cstB], w=[pb2], sig=(tt == 3))
                        T.op("scalar", lambda e: e.activation(out=xh16[db].rearrange("p (t c) -> p t c", t=4)[:, :, ui * 128:(ui + 1) * 128], in_=ps2[:, :].rearrange("p (t c) -> p t c", t=4), func=AF.Copy),
                             r=[pb2], w=[xhB[db]])
                    elif ui == 2:
                        T.op("scalar", lambda e: e.activation(out=BT16[db], in_=cv, func=AF.Silu), r=[cvB], w=[BTB[db]])
                        ps2, pb2 = ps_s()
                        pvb = ps2[:, 0:256].bitcast(BF16)
                        for tt in range(4):
                            T.op("tensor", lambda e, tt=tt: e.transpose(pvb[:, tt * 128:(tt + 1) * 128], BT16[db][:, tt * 128:(tt + 1) * 128], ident16),
                                 r=[BTB[db], identB], w=[pb2], sig=(tt == 3))
                        T.op("vector", lambda e: e.tensor_copy(out=Btok[db], in_=pvb), r=[pb2], w=[BtokB[db]])
                    else:
                        T.op("scalar", lambda e: e.activation(out=CT16[db], in_=cv, func=AF.Silu), r=[cvB], w=[CTB[db]])

                for (w16, ui) in srcs:
                    steps.append(lambda ui=ui: CV(ui))

                def Z():
                    for tt in range(4):
                        t = tc * 4 + tt
                        own = t >= OWN0
                        ps, pb = ps_m()
                        c_lo = 0 if own else 256
                        for kc in range(16):
                            T.op("tensor", lambda e, kc=kc, t=t, c_lo=c_lo: e.matmul(ps[:, c_lo:260], hTv[:, kc, t * 128:(t + 1) * 128], wzdv[:, kc, c_lo:260], start=(kc == 0), stop=(kc == 15)),
                                 r=[wsB, hTB[t]], w=[pb], sig=(kc == 15))
                        T.op("vector", lambda e, tt=tt: e.tensor_tensor(out=dtt[db][:, tt * 4:tt * 4 + 4], in0=ps[:, 256:260], in1=dtb[:, hg * 4:hg * 4 + 4], op=ALU.add), r=[pb, smB], w=[dtB[db]])
                        if own:
                            T.op("scalar", lambda e, tt=tt: e.activation(out=zs[db][:, tt * 256:(tt + 1) * 256], in_=ps[:, 0:256], func=AF.Silu), r=[pb], w=[zsB[db]])
                    T.op("scalar", lambda e: e.activation(out=dtt[db], in_=dtt[db], func=AF.Exp), w=[dtB[db]])
                    T.op("scalar", lambda e: e.activation(out=dtt[db], in_=dtt[db], func=AF.Ln, bias=1.0), w=[dtB[db]])
                steps.append(Z)
                return steps

            def late(tc, hg=hg):
                db = tc % 2
                own_chunk = (tc >= 5)
                steps = []
                A16, Acs16, tot16, etot16, dec16, w16_, eAcs16 = (smv[:, i * 16:(i + 1) * 16] for i in range(7))

                def S():
                    T.op("vector", lambda e: e.tensor_tensor(out=A16.rearrange("p (t r) -> p t r", t=4), in0=dtt[db].rearrange("p (t r) -> p t r", t=4),
                                                             in1=a_neg[:, hg * 4:hg * 4 + 4].unsqueeze(1).to_broadcast([128, 4, 4]), op=ALU.mult),
                         r=[dtB[db], miscB], w=[smvB])
                    ps, pb = ps_m()
                    T.op("tensor", lambda e: e.matmul(ps[:, 0:16], tri, A16, start=True, stop=True), r=[smvB, cstB], w=[pb], sig=False)
                    T.op("tensor", lambda e: e.matmul(ps[:, 16:32], ones32, A16, start=True, stop=True), r=[smvB, cstB], w=[pb])
                    T.op("vector", lambda e: e.tensor_copy(out=smv[:, 16:48], in_=ps[:, 0:32]), r=[pb], w=[smvB])
                    T.op("scalar", lambda e: e.activation(out=etot16, in_=tot16, func=AF.Exp), w=[smvB])
                    T.op("vector", lambda e: e.tensor_tensor(out=dec16, in0=tot16, in1=Acs16, op=ALU.subtract), w=[smvB])
                    T.op("scalar", lambda e: e.activation(out=dec16, in_=dec16, func=AF.Exp), w=[smvB])
                    if own_chunk:
                        T.op("scalar", lambda e: e.activation(out=eAcs16, in_=Acs16, func=AF.Exp), w=[smvB])
                    T.op("vector", lambda e: e.tensor_tensor(out=w16_, in0=dtt[db], in1=dec16, op=ALU.mult), r=[dtB[db]], w=[smvB])
                    T.op("vector", lambda e: e.tensor_tensor(out=w16_.rearrange("p (t r) -> p t r", t=4), in0=w16_.rearrange("p (t r) -> p t r", t=4),
                                                             in1=vtile[:, tc * 4:(tc + 1) * 4].unsqueeze(2).to_broadcast([128, 4, 4]), op=ALU.mult),
                         r=[smB], w=[smvB])
                    T.op("vector", lambda e: e.tensor_tensor(out=Xd.rearrange("p (n c) -> p n c", n=16), in0=xh16[db].rearrange("p (n c) -> p n c", n=16),
                                                             in1=b3(w16_, 16), op=ALU.mult),
                         r=[xhB[db], smvB], w=[XdB])
                steps.append(S)

                def Dst():
                    banks = [ps_s(), ps_s()]
                    for tt in range(4):
                        psd, pbd = banks[tt // 2]
                        c = (tt % 2) * 256
                        T.op("tensor", lambda e, tt=tt, psd=psd, c=c: e.matmul(psd[:, c:c + 256], Btok[db][:, tt * 128:(tt + 1) * 128], Xd[:, tt * 256:(tt + 1) * 256], start=True, stop=True),
                             r=[BtokB[db], XdB], w=[pbd], sig=(tt % 2 == 1))
                    for tt in range(4):
                        t = tc * 4 + tt
                        psd, pbd = banks[tt // 2]
                        c = (tt % 2) * 256
                        if t >= OWN0:
                            T.op("scalar", lambda e, tt=tt: e.activation(out=S16all[:, tt * 256:(tt + 1) * 256], in_=S32, func=AF.Copy), r=[SB_], w=[S16B])
                        T.op("vector", lambda e, tt=tt: e.tensor_tensor(out=S32.rearrange("p (r c) -> p r c", r=4), in0=S32.rearrange("p (r c) -> p r c", r=4),
                                                                        in1=b3(etot16[:, tt * 4:tt * 4 + 4], 4), op=ALU.mult), r=[smvB], w=[SB_])
                        T.op("vector", lambda e, psd=psd, c=c: e.tensor_tensor(out=S32, in0=S32, in1=psd[:, c:c + 256], op=ALU.add), r=[pbd], w=[SB_])
                steps.append(Dst)

                def O(tt):
                    t = tc * 4 + tt
                    qi = t - OWN0
                    so_ = tt % 2
                    lhsD4 = eD4 = lhsD4s[so_]
                    lhsDB = eDB = LDBs[so_]
                    MT4, MTB = MT4s[so_], MTBs[so_]
                    yoff, yoffB, Gm, GmB = yoffs[so_], yoffBs[so_], Gms[so_], GmBs[so_]
                    pso, pbo = ps_s()
                    T.op("tensor", lambda e: e.matmul(pso[:, 0:256], CT16[db][:, tt * 128:(tt + 1) * 128], S16all[:, tt * 256:(tt + 1) * 256], start=True, stop=True),
                         r=[CTB[db], S16B], w=[pbo], sig=False)
                    T.op("tensor", lambda e: e.matmul(pso[:, 256:384], BT16[db][:, tt * 128:(tt + 1) * 128], CT16[db][:, tt * 128:(tt + 1) * 128], start=True, stop=True),
                         r=[BTB[db], CTB[db]], w=[pbo])
                    T.op("vector", lambda e: e.tensor_tensor(out=yoff.rearrange("p (r c) -> p r c", r=4), in0=pso[:, 0:256].rearrange("p (r c) -> p r c", r=4),
                                                             in1=b3(eAcs16[:, tt * 4:tt * 4 + 4], 4), op=ALU.mult), r=[pbo, smvB], w=[yoffB])
                    T.op("vector", lambda e: e.tensor_tensor(out=Gm, in0=pso[:, 256:384], in1=tri, op=ALU.mult), r=[pbo, cstB], w=[GmB])
                    T.op("vector", lambda e: e.tensor_tensor(out=lhsD4.rearrange("p (r c) -> p r c", r=4), in0=umat.unsqueeze(1).to_broadcast([128, 4, 128]),
                                                             in1=A16[:, tt * 4:tt * 4 + 4].unsqueeze(2).to_broadcast([128, 4, 128]), op=ALU.mult),
                         r=[smvB, cstB], w=[lhsDB])
                    psD, pbD = (PS[4 + tt % 2], PB[4 + tt % 2])
                    for r_ in range(4):
                        T.op("tensor", lambda e, r_=r_: e.matmul(psD[:, r_ * 128:(r_ + 1) * 128], lhsD4[:, r_ * 128:(r_ + 1) * 128], tri, start=True, stop=True),
                             r=[lhsDB, cstB], w=[pbD], sig=(r_ == 3))
                    T.op("scalar", lambda e: e.activation(out=eD4, in_=psD[:, :], func=AF.Exp), r=[pbD], w=[eDB])
                    T.op("vector", lambda e: e.tensor_tensor(out=eD4.rearrange("p (r c) -> p r c", r=4), in0=eD4.rearrange("p (r c) -> p r c", r=4),
                                                             in1=Gm.unsqueeze(1).to_broadcast([128, 4, 128]), op=ALU.mult), r=[GmB], w=[eDB])
                    T.op("vector", lambda e: e.tensor_tensor(out=MT4.rearrange("p (r c) -> p r c", r=4), in0=eD4.rearrange("p (r c) -> p r c", r=4),
                                                             in1=dtt[db][:, tt * 4:tt * 4 + 4].unsqueeze(2).to_broadcast([128, 4, 128]), op=ALU.mult),
                         r=[eDB, dtB[db]], w=[MTB])
                    psy, pby = (PS[6], PB[6])
                    for r_ in range(4):
                        T.op("tensor", lambda e, r_=r_: e.matmul(psy[:, r_ * 64:(r_ + 1) * 64], MT4[:, r_ * 128:(r_ + 1) * 128], xh16[db][:, tt * 256 + r_ * 64:tt * 256 + (r_ + 1) * 64], start=True, stop=True),
                             r=[MTB, xhB[db]], w=[pby], sig=(r_ == 3))
                    yo = yg[qi % 2]
                    yoB = ygB[qi % 2]
                    T.op("vector", lambda e: e.tensor_tensor(out=yo, in0=psy[:, 0:256], in1=yoff, op=ALU.add), r=[pby, yoffB], w=[yoB])
                    T.op("vector", lambda e: e.tensor_tensor(out=yoff.rearrange("p (r c) -> p r c", r=4), in0=xh16[db][:, tt * 256:(tt + 1) * 256].rearrange("p (r c) -> p r c", r=4),
                                                             in1=b3(dsk[:, hg * 4:hg * 4 + 4], 4), op=ALU.mult),
                         r=[xhB[db], smB], w=[yoffB])
                    T.op("vector", lambda e: e.tensor_tensor(out=yo, in0=yo, in1=yoff, op=ALU.add), r=[yoffB], w=[yoB])
                    T.op("vector", lambda e: e.tensor_tensor(out=yo, in0=yo, in1=zs[db][:, tt * 256:(tt + 1) * 256], op=ALU.mult), r=[zsB[db]], w=[yoB])
                    T.op("scalar", lambda e: e.activation(out=junk[:, 0:256], in_=yo, func=AF.Square, accum_out=ssq_s[:, qi * 4 + hg:qi * 4 + hg + 1]),
                         r=[yoB], w=[junkB, ssqB])
                    T.dma("scalar", ymix[qi * 128:(qi + 1) * 128, 1024 + hg * 256:1024 + (hg + 1) * 256], yo, yoB, r=[yoB])

                for tt in range(4):
                    if tc * 4 + tt >= OWN0:
                        steps.append(lambda tt=tt: O(tt))
                return steps

            for st_ in early(0):
                st_()
            for tc in range(8):
                L = late(tc)
                E = early(tc + 1) if tc + 1 < 8 else []
                for i in range(max(len(L), len(E))):
                    if i < len(E):
                        E[i]()
                    if i < len(L):
                        L[i]()
        rr["n"] = 3
        phase_done(3)

        A.off = base0
        regX = A.off
        A.off = regX + NCT * 512
        regH = A.off
        h2T = A.bf16(16 * NOWN * 128)
        h2Tv = h2T.rearrange("p (k t) -> p k t", k=16)
        h2TB = Buf("h2T")
        regR = A.off
        A.off = regX
        wo16 = A.bf16(16 * 2048)
        wo16v = wo16.rearrange("p (k c) -> p k c", k=16)
        woB = Buf("wo16")
        wst = A.f32(2048)
        wstB = Buf("wst3")
        Yt = [A.f32(2048), A.f32(2048)]
        YtB = [Buf("Yt0"), Buf("Yt1")]
        assert A.off <= regH, A.off
        A.off = regR
        mx16 = A.bf16(2048)
        mxB = Buf("mx16")
        mxT = A.bf16(2048)
        mxTv = mxT.rearrange("p (k t) -> p k t", k=16)
        mxTB = Buf("mxT")
        xr = A.f32(2048)
        xrB = Buf("xr")
        gate_m = A.f32(2048)
        gs_f = A.f32(2048)
        sh_f = A.f32(2048)
        gfB = Buf("gsf")
        gmB_ = Buf("gate_m")
        h2_16 = A.bf16(2048)
        h2B = Buf("h2_16")
        rs = A.f32(16)
        rsB = Buf("rs")
        junkC = A.bf16(2048)
        w_out_v = w_out.rearrange("(k p) n -> p k n", p=128)
        for kc in range(16):
            T.dma("sync", wst, w_out_v[:, kc, :], wstB, w=[wstB])
            T.op("vector" if kc % 2 else "scalar",
                 (lambda e, kc=kc: e.tensor_scalar(out=wo16v[:, kc, :], in0=wst, scalar1=gcat[:, kc:kc + 1], scalar2=None, op0=ALU.mult)) if kc % 2 else
                 (lambda e, kc=kc: e.activation(out=wo16v[:, kc, :], in_=wst, func=AF.Copy, scale=gcat[:, kc:kc + 1])),
                 r=[wstB, smB], w=[woB])
        bcast_row(gate_m, gmB_, modv[2:3, :])
        load_gs(gs_f, gfB, sh_f, 1, 3, 4, xr, xrB)
        for qi in range(NOWN):
            t = OWN0 + qi
            Y = Yt[qi % 2]
            YB = YtB[qi % 2]
            T.dma("sync", Y, ymix[qi * 128:(qi + 1) * 128, :], YB, w=[YB])
            T.dma("sync", xr, xl_t[t], xrB, w=[xrB])
            T.op("vector", lambda e, qi=qi: e.tensor_reduce(out=rs[:, 0:1], in_=ssq_a[:, qi * 8:(qi + 1) * 8], axis=AX.X, op=ALU.add), r=[ssqB], w=[rsB])
            T.op("vector", lambda e, qi=qi: e.tensor_reduce(out=rs[:, 1:3], in_=ssq_s[:, qi * 4:(qi + 1) * 4].rearrange("p (g c) -> p g c", g=2), axis=AX.X, op=ALU.add), r=[ssqB], w=[rsB])
            T.op("vector", lambda e: e.tensor_scalar(out=rs[:, 0:1], in0=rs[:, 0:1], scalar1=1.0 / 1024, scalar2=EPS, op0=ALU.mult, op1=ALU.add), w=[rsB])
            T.op("vector", lambda e: e.tensor_scalar(out=rs[:, 1:3], in0=rs[:, 1:3], scalar1=1.0 / 512, scalar2=EPS, op0=ALU.mult, op1=ALU.add), w=[rsB])
            T.op("scalar", lambda e: e.activation(out=rs[:, 0:3], in_=rs[:, 0:3], func=AF.Sqrt), w=[rsB])
            T.op("vector", lambda e: e.reciprocal(out=rs[:, 0:3], in_=rs[:, 0:3]), w=[rsB])
            T.op("vector", lambda e, Y=Y: e.tensor_scalar(out=mx16[:, 0:1024], in0=Y[:, 0:1024], scalar1=rs[:, 0:1], scalar2=None, op0=ALU.mult), r=[YB, rsB], w=[mxB])
            T.op("vector", lambda e, Y=Y: e.tensor_scalar(out=mx16[:, 1024:1536], in0=Y[:, 1024:1536], scalar1=rs[:, 1:2], scalar2=None, op0=ALU.mult), r=[YB, rsB], w=[mxB])
            T.op("vector", lambda e, Y=Y: e.tensor_scalar(out=mx16[:, 1536:2048], in0=Y[:, 1536:2048], scalar1=rs[:, 2:3], scalar2=None, op0=ALU.mult), r=[YB, rsB], w=[mxB])
            transpose_to(mx16, mxB, mxTv, mxTB, 0)
            for dc in range(4):
                ps, pb = ACCP[dc]
                for kc in range(16):
                    T.op("tensor", lambda e, ps=ps, kc=kc, dc=dc: e.matmul(ps[:, :], mxTv[:, kc, :], wo16v[:, kc, dc * 512:(dc + 1) * 512], start=(kc == 0), stop=(kc == 15)),
                         r=[mxTB, woB], w=[pb], sig=(kc == 15))
                T.op("vector", lambda e, ps=ps, dc=dc, Y=Y: e.tensor_tensor(out=Y[:, dc * 512:(dc + 1) * 512], in0=ps[:, :], in1=gate_m[:, dc * 512:(dc + 1) * 512], op=ALU.mult),
                     r=[pb, gmB_, mxB], w=[YB])
            T.op("vector", lambda e, Y=Y: e.tensor_tensor(out=xr, in0=xr, in1=Y, op=ALU.add), r=[YB], w=[xrB])
            if qi >= 1:
                T.dma("scalar", xmid[(qi - 1) * 128:qi * 128, :], xr, xrB, r=[xrB])
            norm_tile(xr, xrB, gs_f, sh_f, gfB, vtile[:, t:t + 1], h2_16, h2B, rs[:, 8:16], junkC)
            transpose_to(h2_16, h2B, h2Tv, h2TB, qi * 128)
        phase_done(4)

        A.off = regX
        aT = A.bf16(NCT * 1024)
        aTv = aT.rearrange("p (c t) -> p c t", c=NCT)
        aTB = Buf("aT")
        assert A.off == regH
        A.off = regR
        wu32 = [[A.f32(2048), A.f32(2048)], [A.f32(2048), A.f32(2048)]]
        wu32B = [[Buf("wu32_%d%d" % (a, b)) for b in range(2)] for a in range(2)]
        wu16 = [[A.bf16(2048), A.bf16(2048)], [A.bf16(2048), A.bf16(2048)]]
        wu16B = [[Buf("wu16_%d%d" % (a, b)) for b in range(2)] for a in range(2)]
        ub2 = [A.f32(1026), A.f32(1026)]
        ub2B = [Buf("ub2g"), Buf("ub2v")]
        cv2 = [A.f32(1024), A.f32(1024)]
        cv2B = [Buf("cv2g"), Buf("cv2v")]
        w_up_v = w_up.rearrange("(k p) n -> p k n", p=128)

        def load_up(ct):
            s = ct % 2
            for gv in range(2):
                col0 = gv * FFN + ct * 128
                T.dma("sync", wu32[s][gv].rearrange("p (k c) -> p k c", k=16), w_up_v[:, :, col0:col0 + 128], wu32B[s][gv], w=[wu32B[s][gv]])

        load_up(0)
        for ct in range(NCT):
            s = ct % 2
            if ct + 1 < NCT:
                load_up(ct + 1)
            for gv in range(2):
                if gv == 0:
                    T.op("scalar", lambda e, s=s, gv=gv: e.activation(out=wu16[s][gv], in_=wu32[s][gv], func=AF.Copy), r=[wu32B[s][gv]], w=[wu16B[s][gv]])
                else:
                    T.op("gpsimd", lambda e, s=s, gv=gv: e.tensor_copy(out=wu16[s][gv], in_=wu32[s][gv]), r=[wu32B[s][gv]], w=[wu16B[s][gv]])
            for gv in range(2):
                wv_ = wu16[s][gv].rearrange("p (k c) -> p k c", k=16)
                tile_i = gv * NCT + ct
                eng = "vector"
                ps, pb = ps_s()
                for kc in range(16):
                    T.op("tensor", lambda e, ps=ps, kc=kc, wv_=wv_: e.matmul(ps[:, 0:128], wv_[:, kc, :], h2Tv[:, kc, 0:128], start=(kc == 0), stop=(kc == 15)),
                         r=[wu16B[s][gv], h2TB], w=[pb], sig=(kc == 15))
                T.op("scalar", lambda e, ps=ps, gv=gv: e.activation(out=ub2[gv][:, 0:2], in_=ps[:, 126:128], func=AF.Copy), r=[pb], w=[ub2B[gv]])
                for half in range(2):
                    ps, pb = ps_s()
                    for kc in range(16):
                        T.op("tensor", lambda e, ps=ps, kc=kc, wv_=wv_, half=half: e.matmul(ps[:, :], wv_[:, kc, :], h2Tv[:, kc, 128 + half * 512:128 + (half + 1) * 512], start=(kc == 0), stop=(kc == 15)),
                             r=[wu16B[s][gv], h2TB], w=[pb], sig=(kc == 15))
                    T.op("scalar", lambda e, ps=ps, gv=gv, half=half: e.activation(out=ub2[gv][:, 2 + half * 512:2 + (half + 1) * 512], in_=ps[:, :], func=AF.Copy), r=[pb], w=[ub2B[gv]])
                T.op(eng, lambda e, gv=gv, tile_i=tile_i: e.tensor_scalar(out=cv2[gv], in0=ub2[gv][:, 2:1026], scalar1=cwf[:, tile_i * 3 + 2:tile_i * 3 + 3], scalar2=cbf[:, tile_i:tile_i + 1], op0=ALU.mult, op1=ALU.add),
                     r=[ub2B[gv], smB], w=[cv2B[gv]])
                for jj in range(1, 3):
                    T.op(eng, lambda e, gv=gv, tile_i=tile_i, jj=jj: e.scalar_tensor_tensor(out=cv2[gv], in0=ub2[gv][:, 2 - jj:1026 - jj], scalar=cwf[:, tile_i * 3 + 2 - jj:tile_i * 3 + 3 - jj], in1=cv2[gv], op0=ALU.mult, op1=ALU.add),
                         r=[ub2B[gv], smB], w=[cv2B[gv]])
            T.op("scalar", lambda e: e.activation(out=cv2[0], in_=cv2[0], func=AF.Silu), w=[cv2B[0]])
            T.op("vector", lambda e, ct=ct: e.tensor_tensor(out=aTv[:, ct, :], in0=cv2[0], in1=cv2[1], op=ALU.mult), r=[cv2B[0], cv2B[1]], w=[aTB])
        phase_done(5)

        A.off = regH
        wd32 = [A.f32(NCT * 128), A.f32(NCT * 128)]
        wd32B = [Buf("wd32a"), Buf("wd32b")]
        wd16 = [A.bf16(NCT * 128), A.bf16(NCT * 128)]
        wd16B = [Buf("wd16a"), Buf("wd16b")]
        ffT = [A.f32(512), A.f32(512)]
        ffTB = [Buf("ffT0"), Buf("ffT1")]
        ftok = [A.f32(512), A.f32(512)]
        ftokB = [Buf("ftok0"), Buf("ftok1")]
        w_down_v = w_down.rearrange("(c p) n -> p c n", p=128)
        ffsc_v = ffsc.rearrange("(t p) d -> p t d", p=128)

        def load_wd(dt_):
            s_ = dt_ % 2
            T.dma("sync", wd32[s_].rearrange("p (c n) -> p c n", c=NCT), w_down_v[:, :, dt_ * 128:(dt_ + 1) * 128], wd32B[s_], w=[wd32B[s_]])

        load_wd(0)
        it = 0
        for dt_ in range(16):
            s_ = dt_ % 2
            if dt_ + 1 < 16:
                load_wd(dt_ + 1)
            if s_ == 0:
                T.op("scalar", lambda e, s_=s_: e.activation(out=wd16[s_], in_=wd32[s_], func=AF.Copy), r=[wd32B[s_]], w=[wd16B[s_]])
            else:
                T.op("gpsimd", lambda e, s_=s_: e.tensor_copy(out=wd16[s_], in_=wd32[s_]), r=[wd32B[s_]], w=[wd16B[s_]])
            wdv = wd16[s_].rearrange("p (c n) -> p c n", c=NCT)
            for half in range(2):
                fs = it % 2
                it += 1
                ps, pb = ps_s()
                for ct in range(NCT):
                    T.op("tensor", lambda e, ps=ps, ct=ct, wdv=wdv, half=half: e.matmul(ps[:, :], wdv[:, ct, :], aTv[:, ct, half * 512:(half + 1) * 512], start=(ct == 0), stop=(ct == NCT - 1)),
                         r=[wd16B[s_], aTB], w=[pb], sig=(ct == NCT - 1))
                if fs == 0:
                    T.op("scalar", lambda e, ps=ps, fs=fs: e.activation(out=ffT[fs], in_=ps[:, :], func=AF.Copy), r=[pb], w=[ffTB[fs]])
                else:
                    T.op("vector", lambda e, ps=ps, fs=fs: e.tensor_copy(out=ffT[fs], in_=ps[:, :]), r=[pb], w=[ffTB[fs]])
                ps2, pb2 = ACCP[it % 4]
                for tt in range(4):
                    T.op("tensor", lambda e, ps2=ps2, tt=tt, fs=fs: e.transpose(ps2[:, tt * 128:(tt + 1) * 128], ffT[fs][:, tt * 128:(tt + 1) * 128], ident32),
                         r=[ffTB[fs], cstB], w=[pb2], sig=(tt == 3))
                T.op("vector", lambda e, ps2=ps2, fs=fs: e.tensor_copy(out=ftok[fs], in_=ps2[:, :]), r=[pb2], w=[ftokB[fs]])
                T.dma("scalar", ffsc_v[:, half * 4:(half + 1) * 4, dt_ * 128:(dt_ + 1) * 128], ftok[fs].rearrange("p (t d) -> p t d", t=4), ftokB[fs], r=[ftokB[fs]])
        T.barrier()

        A.off = regH
        fft_ = [A.f32(2048), A.f32(2048)]
        fftB = [Buf("fft0"), Buf("fft1")]
        xm_ = [A.f32(2048), A.f32(2048)]
        xmB_ = [Buf("xm0"), Buf("xm1")]
        gate_f = A.f32(2048)
        fng = A.f32(2048)
        gffB = Buf("gate_f")
        st2 = A.f32(16)
        st2B = Buf("st2")
        bcast_row(gate_f, gffB, modv[5:6, :])
        bcast_row(fng, gffB, vecs[2:3, :])

        def load_fin(tok):
            s_ = tok % 2
            T.dma("sync", fft_[s_], ffsc[tok * 128:(tok + 1) * 128, :], fftB[s_], w=[fftB[s_]])
            T.dma("sync", xm_[s_], xmid[tok * 128:(tok + 1) * 128, :], xmB_[s_], w=[xmB_[s_]])

        load_fin(0)
        for tok in range(8):
            s_ = tok % 2
            if tok + 1 < 8:
                load_fin(tok + 1)
            fft, xm, fB, xB_ = fft_[s_], xm_[s_], fftB[s_], xmB_[s_]
            T.op("gpsimd", lambda e, fft=fft: e.tensor_tensor(out=fft, in0=fft, in1=gate_f, op=ALU.mult), r=[gffB], w=[fB])
            T.op("vector", lambda e, fft=fft, xm=xm: e.tensor_tensor(out=xm, in0=xm, in1=fft, op=ALU.add), r=[fB], w=[xB_])
            T.op("vector", lambda e: e.memset(st2[:, 0:1], 0.0), w=[st2B])
            T.op("scalar", lambda e, fft=fft, xm=xm: e.activation(out=fft, in_=xm, func=AF.Square, accum_out=st2[:, 0:1]), r=[xB_], w=[fB, st2B])
            T.op("vector", lambda e: e.tensor_scalar(out=st2[:, 1:2], in0=st2[:, 0:1], scalar1=1.0 / D, scalar2=EPS, op0=ALU.mult, op1=ALU.add), w=[st2B])
            T.op("scalar", lambda e: e.activation(out=st2[:, 1:2], in_=st2[:, 1:2], func=AF.Sqrt), w=[st2B])
            T.op("vector", lambda e: e.reciprocal(out=st2[:, 1:2], in_=st2[:, 1:2]), w=[st2B])
            T.op("vector", lambda e, xm=xm: e.scalar_tensor_tensor(out=xm, in0=xm, scalar=st2[:, 1:2], in1=fng, op0=ALU.mult, op1=ALU.mult), r=[gffB, st2B], w=[xB_])
            T.dma("scalar", y_out[tok * 128:(tok + 1) * 128, :], xm, xB_, r=[xB_])
        phase_done(6)


    try:
        body()
    except _Stop:
        print('STOPPED after phase', KSTOP)

    engs = {"sync": nc.sync, "scalar": nc.scalar, "vector": nc.vector, "gpsimd": nc.gpsimd, "tensor": nc.tensor}
    with nc.Block() as block:
        @block.sync
        def _(e):
            for f in T.streams["sync"]:
                f(e)

        @block.scalar
        def _(e):
            for f in T.streams["scalar"]:
                f(e)

        @block.vector
        def _(e):
            for f in T.streams["vector"]:
                f(e)

        @block.gpsimd
        def _(e):
            for f in T.streams["gpsimd"]:
                f(e)

        @block.tensor
        def _(e):
            for f in T.streams["tensor"]:
                f(e)
    print("instr counts", {k: len(v) for k, v in T.streams.items()}, "sem counts", T.ecnt, "nsem", T.nsem, "arena hi", A.hi)
    return nc


def rel_bucket_np(dist):
    n = np.maximum(dist, 0)
    max_exact = 16
    nf = np.maximum(n, max_exact).astype(np.float32)
    large = max_exact + (np.log(nf / max_exact) / np.float32(np.log(128 / max_exact)) * (32 - max_exact)).astype(np.int32)
    large = np.minimum(large, 31)
    return np.where(n < max_exact, n, large)


_NC_CACHE = {}


def kernel(x, c, w_ada, b_ada, norm_mix_g, w_in, rel_bias, attn_norm_g, conv_ssm_w, conv_ssm_b,
           dt_bias, a_log, d_skip, ssm_norm_g, w_out, norm_ffn_g, w_up, conv_ffn_w, conv_ffn_b,
           w_down, final_norm_g):
    if "nc" not in _NC_CACHE:
        _NC_CACHE["nc"] = build_nc()
    nc = _NC_CACHE["nc"]
    in_maps = prep_inputs(x, c, w_ada, b_ada, norm_mix_g, w_in, rel_bias, attn_norm_g, conv_ssm_w, conv_ssm_b,
                          dt_bias, a_log, d_skip, ssm_norm_g, w_out, norm_ffn_g, w_up, conv_ffn_w, conv_ffn_b,
                          w_down, final_norm_g)
    res = run_bass_kernel_spmd(nc, in_maps, core_ids=list(range(8)))
    out = np.zeros((2, 4096, D), np.float32)
    for core in range(8):
        b, j = core // 4, core % 4
        out[b, j * 1024:(j + 1) * 1024] = res.results[core]["y"]
    return out


def prep_inputs(x, c, w_ada, b_ada, norm_mix_g, w_in, rel_bias, attn_norm_g, conv_ssm_w, conv_ssm_b,
                dt_bias, a_log, d_skip, ssm_norm_g, w_out, norm_ffn_g, w_up, conv_ffn_w, conv_ffn_b,
                w_down, final_norm_g):
    f = np.float32
    x = np.asarray(x, f)
    consts = np.zeros((128, 512), f)
    k = np.arange(128)
    consts[:, 0:128] = np.eye(128, dtype=f)
    consts[:, 128:256] = (k[:, None] <= k[None, :]).astype(f)
    consts[:, 256:384] = (k[:, None] > k[None, :]).astype(f)
    consts[:, 384:512] = 1.0
    eall = np.zeros((16, 2048), f)
    for n in range(16):
        eall[n, n * 128:(n + 1) * 128] = 1.0
    kk = np.arange(128)[:, None]
    qq = np.arange(512)[None, :]
    btiles = np.zeros((8, 5, 128, 512), f)
    rb = np.asarray(rel_bias, f)
    for oi, off in enumerate(OFFS):
        dist = off + qq - kk
        bk = rel_bucket_np(dist)
        for h in range(8):
            btiles[h, oi] = np.where(dist >= 0, rb[bk, h], f(NEG))
    sm_common = np.zeros((128, 560), f)
    sm_common[:, 64:80] = np.asarray(dt_bias, f)[0][None, :]
    sm_common[:, 80:96] = np.asarray(a_log, f)[0][None, :]
    sm_common[:, 96:112] = np.asarray(d_skip, f)[0][None, :]
    sm_common[:, 112:120] = rb[31][None, :]
    gc = np.concatenate([np.asarray(attn_norm_g, f)[0], np.asarray(ssm_norm_g, f)[0]])
    sm_common[:, 120:136] = gc.reshape(16, 128).T
    cw = np.asarray(conv_ssm_w, f)[0]
    sm_common[:, 136:184] = cw.reshape(4, 12, 128).transpose(2, 1, 0).reshape(128, 48)
    sm_common[:, 184:196] = np.asarray(conv_ssm_b, f)[0].reshape(12, 128).T
    cf = np.asarray(conv_ffn_w, f)[0]
    sm_common[:, 196:460] = cf.reshape(3, 88, 128).transpose(2, 1, 0).reshape(128, 264)
    sm_common[:, 460:548] = np.asarray(conv_ffn_b, f)[0].reshape(88, 128).T
    vecs = np.stack([np.asarray(norm_mix_g, f)[0], np.asarray(norm_ffn_g, f)[0], np.asarray(final_norm_g, f)])
    shared = {
        "consts": consts, "eall": eall, "w_ada": np.ascontiguousarray(np.asarray(w_ada, f)[0]),
        "b_ada": np.ascontiguousarray(np.asarray(b_ada, f)), "vecs": vecs,
        "w_in": np.ascontiguousarray(np.asarray(w_in, f)[0]), "btiles": btiles,
        "w_out": np.ascontiguousarray(np.asarray(w_out, f)[0]), "w_up": np.ascontiguousarray(np.asarray(w_up, f)[0]),
        "w_down": np.ascontiguousarray(np.asarray(w_down, f)[0]),
    }
    in_maps = []
    for core in range(8):
        b, j = core // 4, core % 4
        npad = (3 - j) * 1024
        xl = np.zeros((4096, D), f)
        xl[npad:] = x[b, :(j + 1) * 1024]
        sm = sm_common.copy()
        sm[:, 0:32] = (np.arange(32) * 128 >= npad).astype(f)[None, :]
        sm[:, 32:48] = np.where(np.arange(16) * 256 >= npad, f(0), f(-1e30))[None, :]
        sm[:, 48:64] = np.asarray(c, f)[b].reshape(128, 16)
        d = dict(shared)
        d["xl"] = xl
        d["smalls"] = sm
        in_maps.append(d)
    return in_maps
```

```python
import numpy as np
import concourse.bass as bass
import concourse.mybir as mybir
from concourse.bass_utils import run_bass_kernel_spmd

F32 = mybir.dt.float32
BF16 = mybir.dt.bfloat16
AF = mybir.ActivationFunctionType
ALU = mybir.AluOpType
AX = mybir.AxisListType

D = 2048
NT = 32
OWN0 = 23
NOWN = 9
FFN = 5632
NCT = 44
EPS = 1e-6
NEG = -30000.0
COL_Q, COL_K, COL_V, COL_XS, COL_Z, COL_B, COL_C, COL_DT = 0, 1024, 2048, 3072, 4096, 5120, 5376, 5632
IN_COLS = 5648
OFFS = [-384, -256, -128, 0, 128]
ARENA_WORDS = 53000
import os
KSTOP = int(os.environ.get('KSTOP', '99'))
KSUB = os.environ.get('KSUB', '')
ZERO_INIT = int(os.environ.get('ZERO_INIT', '1'))


class Buf:
    __slots__ = ("name", "w", "r", "dsem", "dcnt", "ex")

    def __init__(self, name, ex=False):
        self.name = name
        self.ex = ex
        self.w = None
        self.r = []
        self.dsem = None
        self.dcnt = 0


class Tracker:
    ENG = ("sync", "scalar", "vector", "gpsimd", "tensor")

    def __init__(self, nc):
        self.nc = nc
        self.streams = {n: [] for n in self.ENG}
        self.esem = {n: nc.alloc_semaphore("es_" + n) for n in self.ENG}
        self.ecnt = {n: 0 for n in self.ENG}
        self.waited = {n: {} for n in self.ENG}
        self.dbufs = []
        self.nsem = 5

    def _wait(self, eng, deps):
        need = {}
        for d in deps:
            if d is None:
                continue
            sem, val, src = d
            if src == eng and eng == "tensor":
                continue
            k = sem.num
            if need.get(k, (None, 0))[1] < val:
                need[k] = (sem, val)
        for k, (sem, val) in need.items():
            if self.waited[eng].get(k, 0) >= val:
                continue
            self.waited[eng][k] = val
            self.streams[eng].append(lambda e, sem=sem, val=val: e.wait_ge(sem, val))

    def op(self, eng, emit, r=(), w=(), sig=True):
        deps = []
        for b in r:
            deps.append(b.w)
            if b.ex:
                deps.extend(d for d in b.r if d[2] != eng)
        for b in w:
            deps.append(b.w)
            deps.extend(b.r)
        self._wait(eng, deps)
        sem = self.esem[eng]
        if sig:
            self.ecnt[eng] += 1
            rec = (sem, self.ecnt[eng], eng)
            self.streams[eng].append(lambda e, emit=emit, sem=sem: emit(e).then_inc(sem, 1))
        else:
            rec = (sem, self.ecnt[eng] + 1, eng)
            self.streams[eng].append(lambda e, emit=emit: emit(e))
        for b in r:
            b.r.append(rec)
        for b in w:
            b.w = rec
            b.r = []

    def dma(self, q, out, in_, sb, r=(), w=()):
        deps = []
        for b in r:
            deps.append(b.w)
        for b in w:
            deps.append(b.w)
            deps.extend(b.r)
        self._wait(q, deps)
        if sb.dsem is None:
            sb.dsem = self.nc.alloc_semaphore("ds_%d" % len(self.dbufs))
            self.dbufs.append(sb)
            self.nsem += 1
        sb.dcnt += 16
        rec = (sb.dsem, sb.dcnt, None)
        sem = sb.dsem
        self.streams[q].append(lambda e, out=out, in_=in_, sem=sem: e.dma_start(out=out, in_=in_).then_inc(sem, 16))
        for b in r:
            b.r.append(rec)
        for b in w:
            b.w = rec
            b.r = []

    def barrier(self):
        deps = [(self.esem[n], self.ecnt[n], n + "_x") for n in self.ENG if self.ecnt[n] > 0]
        deps += [(b.dsem, b.dcnt, None) for b in self.dbufs]
        for n in self.ENG:
            self._wait(n, [d for d in deps if d[2] != n + "_x"])


class Arena:
    def __init__(self, nc):
        self.t = nc.alloc_sbuf_tensor("arena", [128, ARENA_WORDS], F32)
        self.off = 0
        self.hi = 0

    def f32(self, n):
        a = self.t[:, self.off:self.off + n]
        self.off += n
        self.hi = max(self.hi, self.off)
        assert self.off <= ARENA_WORDS, ("arena overflow", self.off)
        return a

    def bf16(self, n):
        w = (n + 1) // 2
        a = self.t[:, self.off:self.off + w].bitcast(BF16)
        self.off += w
        self.hi = max(self.hi, self.off)
        assert self.off <= ARENA_WORDS, ("arena overflow", self.off)
        return a


def build_nc():
    nc = bass.Bass("TRN2", target_bir_lowering=False)

    def din(name, shape):
        return nc.dram_tensor(name, list(shape), F32, kind="ExternalInput").ap()

    xl = din("xl", [4096, D])
    smalls = din("smalls", [128, 560])
    consts = din("consts", [128, 512])
    eall_d = din("eall", [16, 2048])
    w_ada = din("w_ada", [D, 6 * D])
    b_ada = din("b_ada", [1, 6 * D])
    vecs = din("vecs", [3, D])
    w_in = din("w_in", [D, IN_COLS])
    btiles = din("btiles", [8, 5, 128, 512])
    w_out = din("w_out", [D, D])
    w_up = din("w_up", [D, 2 * FFN])
    w_down = din("w_down", [FFN, D])
    y_out = nc.dram_tensor("y", [1024, D], F32, kind="ExternalOutput").ap()
    modv = nc.dram_tensor("modv", [6, D], F32).ap()
    ymix = nc.dram_tensor("ymix", [NOWN * 128, D], F32).ap()
    xmid = nc.dram_tensor("xmid", [1024, D], F32).ap()
    ffsc = nc.dram_tensor("ffsc", [1024, D], F32).ap()

    T = Tracker(nc)
    A = Arena(nc)
    PS = [nc.alloc_psum_tensor("ps%d" % i, [128, 512], F32) for i in range(8)]
    PB = [Buf("ps%d" % i, ex=True) for i in range(8)]
    rr = {"s": 0, "m": 0}
    ACCP = [(PS[3], PB[3]), (PS[4], PB[4]), (PS[5], PB[5]), (PS[6], PB[6])]

    rr["n"] = 3

    def ps_s():
        i = rr["s"] % rr["n"]
        rr["s"] += 1
        return PS[i], PB[i]

    def ps_m():
        i = 7
        return PS[i], PB[i]

    class _Stop(Exception):
        pass

    def phase_done(idx):
        T.barrier()
        if idx == KSTOP:
            raise _Stop()

    def sub_done(tag):
        if tag == KSUB:
            T.barrier()
            raise _Stop()

    def body():
        zB = Buf("zero_init")
        third = ARENA_WORDS // 4
        if ZERO_INIT:
            T.op("vector", lambda e: e.memset(A.t[:, 0:2 * third], 0.0), w=[zB])
            T.op("gpsimd", lambda e: e.memset(A.t[:, 2 * third:3 * third], 0.0), w=[zB])
            T.op("scalar", lambda e: e.activation(out=A.t[:, 3 * third:ARENA_WORDS], in_=A.t[:, 0:ARENA_WORDS - 3 * third], func=AF.Copy), r=[zB], w=[zB])
            for i in range(8):
                T.op("vector", lambda e, i=i: e.memset(PS[i][:, :], 0.0), w=[PB[i]])
            T.barrier()
        cst = A.f32(512)
        cstB = Buf("cst")
        ident32, tri, umat, ones32 = cst[:, 0:128], cst[:, 128:256], cst[:, 256:384], cst[:, 384:512]
        sm = A.f32(560)
        smB = Buf("sm")
        vtile = sm[:, 0:32]
        validneg = sm[:, 32:48]
        c_arr = sm[:, 48:64]
        dtb = sm[:, 64:80]
        alog = sm[:, 80:96]
        dsk = sm[:, 96:112]
        rb31 = sm[:, 112:120]
        gcat = sm[:, 120:136]
        cw_ssm = sm[:, 136:184]
        cb_ssm = sm[:, 184:196]
        cwf = sm[:, 196:460]
        cbf = sm[:, 460:548]
        ident16 = A.bf16(128)
        eall16 = A.bf16(2048)
        misc = A.f32(128)
        miscB = Buf("misc")
        sc_silu = misc[:, 0:16]
        a_neg = misc[:, 16:32]
        mk5 = misc[:, 32:112]
        ssq_a = A.f32(NOWN * 8)
        ssq_s = A.f32(NOWN * 4)
        ssqB = Buf("ssq")
        junk = A.bf16(256)
        junkB = Buf("junk")
        stat = A.f32(16)
        statB = Buf("stat")

        T.dma("sync", cst, consts[:, :], cstB, w=[cstB])
        T.dma("sync", sm, smalls[:, :], smB, w=[smB])
        e32 = A.f32(2048)
        e32B = Buf("e32")
        T.dma("sync", e32[0:16, :], eall_d[:, :], e32B, w=[e32B])
        identB = Buf("ident16")
        T.op("vector", lambda e: e.tensor_copy(out=ident16, in_=ident32), r=[cstB], w=[identB])
        T.op("vector", lambda e: e.tensor_copy(out=eall16[0:16, :], in_=e32[0:16, :]), r=[e32B], w=[identB])
        T.op("scalar", lambda e: e.activation(out=sc_silu, in_=c_arr, func=AF.Silu), r=[smB], w=[miscB])
        T.op("scalar", lambda e: e.activation(out=a_neg, in_=alog, func=AF.Exp), r=[smB], w=[miscB])
        T.op("vector", lambda e: e.tensor_scalar(out=a_neg, in0=a_neg, scalar1=-1.0, scalar2=None, op0=ALU.mult), r=[miscB], w=[miscB])
        for i in range(5):
            blk = 11 + i
            T.op("vector", lambda e, i=i: e.tensor_copy(out=mk5[:, i * 16:(i + 1) * 16], in_=validneg), r=[smB], w=[miscB])
            T.op("vector", lambda e, i=i, blk=blk: e.memset(mk5[:, i * 16 + blk:(i + 1) * 16], -1e30), w=[miscB])
        T.op("vector", lambda e: e.memset(ssq_a, 0.0), w=[ssqB])
        T.op("vector", lambda e: e.memset(ssq_s, 0.0), w=[ssqB])
        A.off -= 2048
        base0 = A.off

        NWA = 6
        wA = [A.f32(2048) for _ in range(NWA)]
        wAB = [Buf("wA%d" % i) for i in range(NWA)]
        tmp0 = A.f32(2048)
        tmp0B = Buf("tmp0")
        T.barrier()
        w_ada_v = w_ada.rearrange("(p k) n -> p k n", k=16)
        cnt = 0
        for m in range(2):
            T.dma("sync", tmp0[0:1, :], b_ada[0:1, m * D:(m + 1) * D], tmp0B, w=[tmp0B])
            for k in range(16):
                sl = cnt % NWA
                q = "sync" if cnt % 2 == 0 else "scalar"
                cnt += 1
                T.dma(q, wA[sl], w_ada_v[:, k, m * D:(m + 1) * D], wAB[sl], w=[wAB[sl]])
                for cchunk in range(4):
                    aps, apb = ACCP[cchunk]
                    T.op("tensor", lambda e, aps=aps, sl=sl, k=k, cchunk=cchunk: e.matmul(aps[0:1, :], sc_silu[:, k:k + 1], wA[sl][:, cchunk * 512:(cchunk + 1) * 512], start=(k == 0), stop=(k == 15)),
                         r=[wAB[sl], miscB], w=[apb], sig=(cchunk == 3))
            for cchunk in range(4):
                aps, apb = ACCP[cchunk]
                T.op("vector", lambda e, aps=aps, cchunk=cchunk: e.tensor_tensor(out=tmp0[0:1, cchunk * 512:(cchunk + 1) * 512], in0=aps[0:1, :], in1=tmp0[0:1, cchunk * 512:(cchunk + 1) * 512], op=ALU.add),
                     r=[apb], w=[tmp0B])
            T.dma("scalar", modv[m:m + 1, :], tmp0[0:1, :], tmp0B, r=[tmp0B])
        phase_done(0)

        def bcast_row(dst, dstB, src_row):
            T.dma("sync", dst, src_row.partition_broadcast(128)[:, 0, :], dstB, w=[dstB])

        A.off = base0
        hT = A.bf16(16 * 4096)
        hTv = hT.rearrange("p (k t) -> p k t", k=16)
        hTB = [Buf("hT%d" % t) for t in range(NT)]
        baseB = A.off
        gs_m = A.f32(2048)
        sh_m = A.f32(2048)
        gsB = Buf("gs_m")
        xt = [A.f32(2048), A.f32(2048)]
        xtB = [Buf("xt0"), Buf("xt1")]
        junkbig = A.bf16(2048)
        h16 = [A.bf16(2048), A.bf16(2048)]
        h16B = [Buf("h16a"), Buf("h16b")]

        def load_gs(gs, gsB_, sh, g_row, m_shift, m_scale, tmpap, tmpB):
            bcast_row(tmpap, tmpB, vecs[g_row:g_row + 1, :])
            bcast_row(gs, gsB_, modv[m_scale:m_scale + 1, :])
            bcast_row(sh, gsB_, modv[m_shift:m_shift + 1, :])
            T.op("vector", lambda e: e.scalar_tensor_tensor(out=gs, in0=gs, scalar=1.0, in1=tmpap, op0=ALU.add, op1=ALU.mult),
                 r=[tmpB], w=[gsB_])

        load_gs(gs_m, gsB, sh_m, 0, 0, 1, xt[0], xtB[0])
        wA2 = [A.f32(2048), A.f32(2048)]
        wA2B = [Buf("wA2a"), Buf("wA2b")]
        tmpb = A.f32(1024)
        tmpbB = Buf("tmpb")
        gsteps = []
        for m in range(2, 6):
            for k in range(16):
                def kstep(m=m, k=k):
                    sl = (m * 16 + k) % 2
                    T.dma("gpsimd", wA2[sl], w_ada_v[:, k, m * D:(m + 1) * D], wA2B[sl], w=[wA2B[sl]])
                    for cchunk in range(4):
                        aps, apb = ACCP[cchunk]
                        T.op("tensor", lambda e, aps=aps, cchunk=cchunk: e.matmul(aps[0:1, :], sc_silu[:, k:k + 1], wA2[sl][:, cchunk * 512:(cchunk + 1) * 512], start=(k == 0), stop=(k == 15)),
                             r=[wA2B[sl], miscB], w=[apb], sig=(cchunk == 3))
                    if k == 15:
                        for half in range(2):
                            T.dma("gpsimd", tmpb[0:1, :], b_ada[0:1, m * D + half * 1024:m * D + (half + 1) * 1024], tmpbB, w=[tmpbB])
                            for c2 in range(2):
                                aps, apb = ACCP[half * 2 + c2]
                                T.op("vector", lambda e, aps=aps, c2=c2: e.tensor_tensor(out=tmpb[0:1, c2 * 512:(c2 + 1) * 512], in0=aps[0:1, :], in1=tmpb[0:1, c2 * 512:(c2 + 1) * 512], op=ALU.add),
                                     r=[apb], w=[tmpbB])
                            T.dma("gpsimd", modv[m:m + 1, half * 1024:(half + 1) * 1024], tmpb[0:1, :], tmpbB, r=[tmpbB])
                gsteps.append(kstep)

        def norm_tile(x_ap, xB, gs, sh, gsB_, vcol, out16, out16B, st, junkbig):
            T.op("vector", lambda e: e.memset(st[:, 0:1], 0.0), w=[statB])
            T.op("scalar", lambda e: e.activation(out=junkbig, in_=x_ap, func=AF.Square, accum_out=st[:, 0:1]), r=[xB], w=[junkB, statB])
            T.op("vector", lambda e: e.tensor_scalar(out=st[:, 1:2], in0=st[:, 0:1], scalar1=1.0 / D, scalar2=EPS, op0=ALU.mult, op1=ALU.add), w=[statB])
            T.op("scalar", lambda e: e.activation(out=st[:, 1:2], in_=st[:, 1:2], func=AF.Sqrt), w=[statB])
            T.op("vector", lambda e: e.reciprocal(out=st[:, 1:2], in_=st[:, 1:2]), w=[statB])
            if vcol is not None:
                T.op("vector", lambda e: e.tensor_tensor(out=st[:, 1:2], in0=st[:, 1:2], in1=vcol, op=ALU.mult), r=[smB], w=[statB])
            T.op("vector", lambda e: e.scalar_tensor_tensor(out=x_ap, in0=x_ap, scalar=st[:, 1:2], in1=gs, op0=ALU.mult, op1=ALU.mult),
                 r=[gsB_, statB], w=[xB])
            if vcol is not None:
                T.op("vector", lambda e: e.scalar_tensor_tensor(out=out16, in0=sh, scalar=vcol, in1=x_ap, op0=ALU.mult, op1=ALU.add),
                     r=[gsB_, xB, smB], w=[out16B])
            else:
                T.op("vector", lambda e: e.tensor_tensor(out=out16, in0=sh, in1=x_ap, op=ALU.add), r=[gsB_, xB], w=[out16B])

        def transpose_to(src16, srcB, dstv, dstB, tok0, ntok=128):
            for half in range(2):
                ps, pb = ps_s()
                pv = ps[:, :].bitcast(BF16).rearrange("p (k t) -> p k t", k=8)
                for kk in range(8):
                    kc = half * 8 + kk
                    T.op("tensor", lambda e, pv=pv, kk=kk, kc=kc: e.transpose(pv[:, kk, :], src16[:, kc * 128:(kc + 1) * 128], ident16),
                         r=[srcB, identB], w=[pb], sig=(kk == 7))
                eng = "scalar" if half == 0 else "vector"
                if eng == "scalar":
                    T.op(eng, lambda e, pv=pv, half=half: e.activation(out=dstv[:, half * 8:(half + 1) * 8, tok0:tok0 + 128], in_=pv, func=AF.Copy), r=[pb], w=[dstB])
                else:
                    T.op(eng, lambda e, pv=pv, half=half: e.tensor_copy(out=dstv[:, half * 8:(half + 1) * 8, tok0:tok0 + 128], in_=pv), r=[pb], w=[dstB])

        xl_t = xl.rearrange("(t p) d -> t p d", p=128)
        T.dma("sync", xt[1], xl_t[0], xtB[1], w=[xtB[1]])
        for t in range(NT):
            sl = (t + 1) % 2
            if t + 1 < NT:
                T.dma("sync", xt[t % 2], xl_t[t + 1], xtB[t % 2], w=[xtB[t % 2]])
            norm_tile(xt[sl], xtB[sl], gs_m, sh_m, gsB, vtile[:, t:t + 1], h16[t % 2], h16B[t % 2], stat, junkbig)
            transpose_to(h16[t % 2], h16B[t % 2], hTv, hTB[t], t * 128)
            for _ in range(2):
                if gsteps:
                    gsteps.pop(0)()
        assert not gsteps
        phase_done(1)

        A.off = baseB
        w_in_v = w_in.rearrange("(k p) n -> p k n", p=128)
        wst = A.f32(2048)
        wstB = Buf("wst")
        wq16, wk16, wv16 = A.bf16(2048), A.bf16(2048), A.bf16(2048)
        wqB, wkB, wvB = Buf("wq"), Buf("wk"), Buf("wv")
        KT = A.bf16(4096)
        KTB = Buf("KT")
        Vaug = A.bf16(NT * 130)
        Vv = Vaug.rearrange("p (t c) -> p t c", c=130)
        VB = Buf("V")
        q32 = A.f32(NOWN * 128)
        q16 = A.bf16(NOWN * 128)
        qB = Buf("q")
        negmT = A.bf16(NOWN * 128)
        negB = Buf("negmT")
        ksum = A.f32(16)
        ksB = Buf("ksum")
        PT = [A.bf16(512), A.bf16(512)]
        PTB = [Buf("PT0"), Buf("PT1")]
        ssb = [A.f32(512), A.f32(512)]
        ssbB = [Buf("ssb0"), Buf("ssb1")]
        biasT = A.f32(5 * 512)
        biasB = Buf("biasT")
        osb = [A.f32(128), A.f32(128)]
        osbB = [Buf("o0"), Buf("o1")]
        gw = A.f32(64)
        gwB = Buf("gw")
        negm2 = [A.f32(16), A.f32(16)]
        negm2B = [Buf("negm0"), Buf("negm1")]
        endB1 = A.off
        T.op("vector", lambda e: e.memset(Vaug, 1.0), w=[VB])

        def load_w(dst16, dstB, col0, ncols=128, eng="vector"):
            T.dma("sync", wst.rearrange("p (k c) -> p k c", k=16)[:, :, 0:ncols], w_in_v[:, :, col0:col0 + ncols], wstB, w=[wstB])
            dv = dst16.rearrange("p (k c) -> p k c", k=16)[:, :, 0:ncols] if ncols != 128 else dst16.rearrange("p (k c) -> p k c", k=16)
            sv = wst.rearrange("p (k c) -> p k c", k=16)[:, :, 0:ncols]
            if eng == "vector":
                T.op("vector", lambda e: e.tensor_copy(out=dv, in_=sv), r=[wstB], w=[dstB])
            else:
                T.op("scalar", lambda e: e.activation(out=dv, in_=sv, func=AF.Copy), r=[wstB], w=[dstB])

        chunks = [(23, 1), (24, 4), (28, 4)]
        SCALE = 128 ** -0.5
        kslot = 0
        for h in range(8):
            load_w(wk16, wkB, COL_K + h * 128, eng="vector")
            load_w(wq16, wqB, COL_Q + h * 128, eng="scalar")
            load_w(wv16, wvB, COL_V + h * 128, eng="vector")
            T.dma("sync", biasT.rearrange("p (o q) -> p o q", o=5), btiles[h].rearrange("o k q -> k o q"), biasB, w=[biasB])
            wkv = wk16.rearrange("p (k c) -> p k c", k=16)
            wvv = wv16.rearrange("p (k c) -> p k c", k=16)
            wqv = wq16.rearrange("p (k c) -> p k c", k=16)
            sub_done("ld%d" % h)
            for tc in range(8):
                ps, pb = ps_s()
                for kc in range(16):
                    T.op("tensor", lambda e, ps=ps, kc=kc, tc=tc: e.matmul(ps[:, :], wkv[:, kc, :], hTv[:, kc, tc * 512:(tc + 1) * 512], start=(kc == 0), stop=(kc == 15)),
                         r=[wkB] + hTB[tc * 4:tc * 4 + 4], w=[pb], sig=(kc == 15))
                T.op("scalar", lambda e, ps=ps, tc=tc: e.activation(out=KT[:, tc * 512:(tc + 1) * 512], in_=ps[:, :], func=AF.Copy), r=[pb], w=[KTB])
                T.op("vector", lambda e, ps=ps, tc=tc: e.tensor_reduce(out=ksum[:, tc * 2:tc * 2 + 2], in_=ps[:, :].rearrange("p (b t) -> p b t", b=2), axis=AX.X, op=ALU.add),
                     r=[pb], w=[ksB])
            sub_done("k%d" % h)
            for (qt0, nq) in chunks:
                ps, pb = ps_s()
                W = nq * 128
                c0 = (qt0 - OWN0) * 128
                for kc in range(16):
                    T.op("tensor", lambda e, ps=ps, kc=kc, qt0=qt0, W=W: e.matmul(ps[:, 0:W], wqv[:, kc, :], hTv[:, kc, qt0 * 128:qt0 * 128 + W], start=(kc == 0), stop=(kc == 15)),
                         r=[wqB] + hTB[qt0:qt0 + nq], w=[pb], sig=(kc == 15))
                T.op("scalar", lambda e, ps=ps, c0=c0, W=W: e.activation(out=q32[:, c0:c0 + W], in_=ps[:, 0:W], func=AF.Copy, scale=SCALE), r=[pb], w=[qB])
                T.op("vector", lambda e, c0=c0, W=W: e.tensor_copy(out=q16[:, c0:c0 + W], in_=q32[:, c0:c0 + W]), r=[qB], w=[qB])
            sub_done("proj%d" % h)

            def gate_mm(qi):
                qt = OWN0 + qi
                blk = qt // 2
                mi = blk - 11
                ng = negm2[qi % 2]
                ps, pb = ps_m()
                T.op("tensor", lambda e: e.matmul(ps[:, 0:16], q32[:, qi * 128:(qi + 1) * 128], ksum, start=True, stop=True), r=[qB, ksB], w=[pb])
                T.op("vector", lambda e: e.tensor_tensor(out=gw[:, 0:16], in0=ps[:, 0:16], in1=mk5[:, mi * 16:(mi + 1) * 16], op=ALU.add), r=[pb, miscB], w=[gwB])
                T.op("vector", lambda e: e.max(out=gw[:, 16:24], in_=gw[:, 0:16]), w=[gwB])
                T.op("vector", lambda e: e.tensor_scalar(out=gw[:, 24:25], in0=gw[:, 18:19], scalar1=-1e29, scalar2=None, op0=ALU.max), w=[gwB])
                T.op("vector", lambda e: e.tensor_scalar(out=gw[:, 32:48], in0=gw[:, 0:16], scalar1=gw[:, 24:25], scalar2=None, op0=ALU.is_ge), w=[gwB])
                T.op("vector", lambda e: e.tensor_scalar(out=ng, in0=gw[:, 32:48], scalar1=-1.0, scalar2=-NEG, op0=ALU.add, op1=ALU.mult), r=[gwB], w=[negm2B[qi % 2]])
                T.op("vector", lambda e: e.memset(ng[:, blk:blk + 1], 0.0), w=[negm2B[qi % 2]])

            def gate_tr(qi):
                ng = negm2[qi % 2]
                ps2, pb2 = ps_m()
                T.op("tensor", lambda e: e.transpose(ps2[0:16, 128:256], ng, ident32), r=[negm2B[qi % 2], cstB], w=[pb2])
                T.op("vector", lambda e: e.tensor_copy(out=negmT[0:16, qi * 128:(qi + 1) * 128], in_=ps2[0:16, 128:256]), r=[pb2], w=[negB])

            for tg in range(8):
                ps, pb = ps_s()
                for tt in range(4):
                    t = tg * 4 + tt
                    for kc in range(16):
                        T.op("tensor", lambda e, ps=ps, kc=kc, t=t, tt=tt: e.matmul(ps[:, tt * 128:(tt + 1) * 128], hTv[:, kc, t * 128:(t + 1) * 128], wvv[:, kc, :], start=(kc == 0), stop=(kc == 15)),
                             r=[wvB, hTB[t]], w=[pb], sig=(kc == 15 and tt == 3))
                T.op("vector", lambda e, ps=ps, tg=tg: e.tensor_copy(out=Vv[:, tg * 4:(tg + 1) * 4, 0:128], in_=ps[:, :].rearrange("p (t c) -> p t c", t=4)), r=[pb], w=[VB])
                gate_mm(tg)
                if tg >= 1:
                    gate_tr(tg - 1)
            gate_mm(8)
            gate_tr(7)
            gate_tr(8)
            sub_done("gate%d" % h)
            for (qt0, nq) in chunks:
                W = nq * 128
                c0 = (qt0 - OWN0) * 128
                lastblk = (qt0 + nq - 1) // 2
                nkt = 2 * lastblk + 2

                def emit_S(kt, c0=c0, W=W):
                    ps, pb = ps_s()
                    kb = kt // 2
                    T.op("tensor", lambda e, ps=ps, kt=kt: e.matmul(ps[:, 0:W], KT[:, kt * 128:(kt + 1) * 128], q16[:, c0:c0 + W], start=True, stop=False),
                         r=[KTB, qB], w=[pb], sig=False)
                    T.op("tensor", lambda e, ps=ps, kb=kb: e.matmul(ps[:, 0:W], eall16[0:16, kb * 128:(kb + 1) * 128], negmT[0:16, c0:c0 + W], start=False, stop=True),
                         r=[negB, identB], w=[pb])
                    return ps, pb

                def emit_E(kt, ps, pb, sl, W=W, qt0=qt0, h=h):
                    off = qt0 * 128 - kt * 128
                    if off <= 128:
                        oi = OFFS.index(off)
                        T.op("vector", lambda e: e.tensor_tensor(out=ssb[sl][:, 0:W], in0=ps[:, 0:W], in1=biasT[:, oi * 512:oi * 512 + W], op=ALU.add),
                             r=[pb, biasB], w=[ssbB[sl]])
                        T.op("scalar", lambda e: e.activation(out=PT[sl][:, 0:W], in_=ssb[sl][:, 0:W], func=AF.Exp), r=[ssbB[sl]], w=[PTB[sl]])
                    else:
                        T.op("scalar", lambda e: e.activation(out=PT[sl][:, 0:W], in_=ps[:, 0:W], func=AF.Exp, bias=rb31[:, h:h + 1]),
                             r=[pb, smB], w=[PTB[sl]])

                def emit_PV(kt, sl, nq=nq, qt0=qt0):
                    kb = kt // 2
                    for j in range(nq):
                        qblk = (qt0 + j) // 2
                        if kb > qblk:
                            continue
                        aps, apb = ACCP[j]
                        last = (kt == 2 * qblk + 1)
                        T.op("tensor", lambda e, aps=aps, j=j, last=last: e.matmul(aps[:, 0:129], PT[sl][:, j * 128:(j + 1) * 128], Vv[:, kt, 0:129], start=(kt == 0), stop=last),
                             r=[PTB[sl], VB], w=[apb], sig=last)

                cur = emit_S(0)
                for kt in range(nkt):
                    nxt = emit_S(kt + 1) if kt + 1 < nkt else None
                    sl = kslot % 2
                    kslot += 1
                    emit_E(kt, cur[0], cur[1], sl)
                    emit_PV(kt, sl)
                    cur = nxt
                sub_done("sc%d_%d" % (h, qt0))
                for j in range(nq):
                    qi = qt0 + j - OWN0
                    aps, apb = ACCP[j]
                    so = (qi + h) % 2
                    T.op("vector", lambda e, aps=aps: e.reciprocal(out=gw[:, 56:57], in_=aps[:, 128:129]), r=[apb], w=[gwB])
                    T.op("scalar", lambda e, aps=aps, so=so: e.activation(out=osb[so], in_=aps[:, 0:128], func=AF.Copy, scale=gw[:, 56:57]), r=[apb, gwB], w=[osbB[so]])
                    T.op("scalar", lambda e, so=so, qi=qi, h=h: e.activation(out=junk[:, 0:128], in_=osb[so], func=AF.Square, accum_out=ssq_a[:, qi * 8 + h:qi * 8 + h + 1]),
                         r=[osbB[so]], w=[junkB, ssqB])
                    T.dma("scalar", ymix[qi * 128:(qi + 1) * 128, h * 128:(h + 1) * 128], osb[so], osbB[so], r=[osbB[so]])
        phase_done(2)

        A.off = baseB
        wst = A.f32(2048)
        wstB = Buf("wst2")
        lhsD4s = [wst[:, 0:512], wst[:, 768:1280]]
        MT4s = [wst[:, 512:768].bitcast(BF16), wst[:, 1280:1536].bitcast(BF16)]
        LDBs = [Buf("lhsD4a"), Buf("lhsD4b")]
        MTBs = [Buf("MT4a"), Buf("MT4b")]
        wx16 = [A.bf16(2048), A.bf16(2048)]
        wb16, wc16 = A.bf16(2048), A.bf16(2048)
        wzd16 = A.bf16(16 * 260)
        wsB = Buf("w_ssd")
        ubuf = [A.f32(516) for _ in range(4)]
        ubB = [Buf("ub%d" % i) for i in range(4)]
        cv = A.f32(512)
        cvB = Buf("cv")
        xhT = A.f32(512)
        xhTB = Buf("xhT")
        BT16 = [A.bf16(512), A.bf16(512)]
        CT16 = [A.bf16(512), A.bf16(512)]
        BTB = [Buf("BT0"), Buf("BT1")]
        CTB = [Buf("CT0"), Buf("CT1")]
        xh16 = [A.bf16(1024), A.bf16(1024)]
        xhB = [Buf("xh0"), Buf("xh1")]
        Btok = [A.bf16(512), A.bf16(512)]
        BtokB = [Buf("Btok0"), Buf("Btok1")]
        zs = [A.bf16(1024), A.bf16(1024)]
        zsB = [Buf("zs0"), Buf("zs1")]
        dtt = [A.f32(16), A.f32(16)]
        dtB = [Buf("dt0"), Buf("dt1")]
        S32 = A.f32(256)
        S16all = A.bf16(1024)
        SB_ = Buf("S")
        S16B = Buf("S16")
        smv = A.f32(128)
        smvB = Buf("smv")
        Xd = A.bf16(1024)
        XdB = Buf("Xd")
        yoffs = [A.f32(256), A.f32(256)]
        yoffBs = [Buf("yoff0"), Buf("yoff1")]
        Gms = [A.f32(128), A.f32(128)]
        GmBs = [Buf("Gm0"), Buf("Gm1")]
        yg = [A.f32(256), A.f32(256)]
        ygB = [Buf("yg0"), Buf("yg1")]
        assert A.off <= ARENA_WORDS

        def load_w2(dst16, col0, ncols, c_off=0):
            wv_ = wst.rearrange("p (k c) -> p k c", k=16)[:, :, 0:ncols]
            T.dma("sync", wv_, w_in_v[:, :, col0:col0 + ncols], wstB, w=[wstB])
            dv = dst16.rearrange("p (k c) -> p k c", k=16)[:, :, c_off:c_off + ncols]
            T.op("vector", lambda e: e.tensor_copy(out=dv, in_=wv_), r=[wstB], w=[wsB])

        def b3(ap, n):
            return ap.unsqueeze(2).to_broadcast([128, n, 64])

        rr["n"] = 4
        for hg in range(4):
            g = hg // 2
            T.barrier()
            load_w2(wx16[0], COL_XS + hg * 256, 128)
            load_w2(wx16[1], COL_XS + hg * 256 + 128, 128)
            load_w2(wb16, COL_B + g * 128, 128)
            load_w2(wc16, COL_C + g * 128, 128)
            load_w2(wzd16, COL_Z + hg * 256, 128, c_off=0)
            load_w2(wzd16, COL_Z + hg * 256 + 128, 128, c_off=128)
            load_w2(wzd16, COL_DT + hg * 4, 4, c_off=256)
            T.barrier()
            wzdv = wzd16.rearrange("p (k c) -> p k c", k=16)
            T.op("vector", lambda e: e.memset(S32, 0.0), w=[SB_])
            for i in range(4):
                T.op("vector", lambda e, i=i: e.memset(ubuf[i], 0.0), w=[ubB[i]])
            ctile = [hg * 2, hg * 2 + 1, 8 + g, 10 + g]

            def early(tc, hg=hg, ctile=ctile, wzdv=wzdv):
                db = tc % 2
                own_chunk = (tc >= 5)
                steps = []
                srcs = [(wx16[0], 0), (wx16[1], 1), (wb16, 2)] + ([(wc16, 3)] if own_chunk else [])

                def MM():
                    for (w16, ui) in srcs:
                        wv_ = w16.rearrange("p (k c) -> p k c", k=16)
                        ps, pb = ps_s()
                        for kc in range(16):
                            T.op("tensor", lambda e, kc=kc, ps=ps, wv_=wv_: e.matmul(ps[:, :], wv_[:, kc, :], hTv[:, kc, tc * 512:(tc + 1) * 512], start=(kc == 0), stop=(kc == 15)),
                                 r=[wsB] + hTB[tc * 4:tc * 4 + 4], w=[pb], sig=(kc == 15))
                        ub = ubuf[ui]
                        T.op("vector", lambda e, ub=ub: e.tensor_copy(out=ub[:, 1:4], in_=ub[:, 513:516]), w=[ubB[ui]])
                        T.op("scalar", lambda e, ub=ub, ps=ps: e.activation(out=ub[:, 4:516], in_=ps[:, :], func=AF.Copy), r=[pb], w=[ubB[ui]])
                steps.append(MM)

                def CV(ui):
                    ub = ubuf[ui]
                    ct = ctile[ui]
                    T.op("vector", lambda e: e.tensor_scalar(out=cv, in0=ub[:, 4:516], scalar1=cw_ssm[:, ct * 4 + 3:ct * 4 + 4], scalar2=cb_ssm[:, ct:ct + 1], op0=ALU.mult, op1=ALU.add),
                         r=[ubB[ui], smB], w=[cvB])
                    for jj in range(1, 4):
                        T.op("vector", lambda e, jj=jj: e.scalar_tensor_tensor(out=cv, in0=ub[:, 4 - jj:516 - jj], scalar=cw_ssm[:, ct * 4 + 3 - jj:ct * 4 + 4 - jj], in1=cv, op0=ALU.mult, op1=ALU.add),
                             r=[ubB[ui], smB], w=[cvB])
                    if ui < 2:
                        T.op("scalar", lambda e: e.activation(out=xhT, in_=cv, func=AF.Silu), r=[cvB], w=[xhTB])
                        ps2, pb2 = ps_s()
                        for tt in range(4):
                            T.op("tensor", lambda e, tt=tt: e.transpose(ps2[:, tt * 128:(tt + 1) * 128], xhT[:, tt * 128:(tt + 1) * 128], ident32),
                                 r=[xhTB, cstB], w=[pb2], sig=(tt == 3))
                        T.op("scalar", lambda e: e.activation(out=xh16[db].rearrange("p (t c) -> p t c", t=4)[:, :, ui * 128:(ui + 1) * 128], in_=ps2[:, :].rearrange("p (t c) -> p t c", t=4), func=AF.Copy),
                             r=[pb2], w=[xhB[db]])
                    elif ui == 2:
                        T.op("scalar", lambda e: e.activation(out=BT16[db], in_=cv, func=AF.Silu), r=[cvB], w=[BTB[db]])
                        ps2, pb2 = ps_s()
                        pvb = ps2[:, 0:256].bitcast(BF16)
                        for tt in range(4):
                            T.op("tensor", lambda e, tt=tt: e.transpose(pvb[:, tt * 128:(tt + 1) * 128], BT16[db][:, tt * 128:(tt + 1) * 128], ident16),
                                 r=[BTB[db], identB], w=[pb2], sig=(tt == 3))
                        T.op("vector", lambda e: e.tensor_copy(out=Btok[db], in_=pvb), r=[pb2], w=[BtokB[db]])
                    else:
                        T.op("scalar", lambda e: e.activation(out=CT16[db], in_=cv, func=AF.Silu), r=[cvB], w=[CTB[db]])

                for (w16, ui) in srcs:
                    steps.append(lambda ui=ui: CV(ui))

                def Z():
                    for tt in range(4):
                        t = tc * 4 + tt
                        own = t >= OWN0
                        ps, pb = ps_m()
                        c_lo = 0 if own else 256
                        for kc in range(16):
                            T.op("tensor", lambda e, kc=kc, t=t, c_lo=c_lo: e.matmul(ps[:, c_lo:260], hTv[:, kc, t * 128:(t + 1) * 128], wzdv[:, kc, c_lo:260], start=(kc == 0), stop=(kc == 15)),
                                 r=[wsB, hTB[t]], w=[pb], sig=(kc == 15))
                        T.op("vector", lambda e, tt=tt: e.tensor_tensor(out=dtt[db][:, tt * 4:tt * 4 + 4], in0=ps[:, 256:260], in1=dtb[:, hg * 4:hg * 4 + 4], op=ALU.add), r=[pb, smB], w=[dtB[db]])
                        if own:
                            T.op("scalar", lambda e, tt=tt: e.activation(out=zs[db][:, tt * 256:(tt + 1) * 256], in_=ps[:, 0:256], func=AF.Silu), r=[pb], w=[zsB[db]])
                    T.op("scalar", lambda e: e.activation(out=dtt[db], in_=dtt[db], func=AF.Exp), w=[dtB[db]])
                    T.op("scalar", lambda e: e.activation(out=dtt[db], in_=dtt[db], func=AF.Ln, bias=1.0), w=[dtB[db]])
                steps.append(Z)
                return steps

            def late(tc, hg=hg):
                db = tc % 2
                own_chunk = (tc >= 5)
                steps = []
                A16, Acs16, tot16, etot16, dec16, w16_, eAcs16 = (smv[:, i * 16:(i + 1) * 16] for i in range(7))

                def S():
                    T.op("vector", lambda e: e.tensor_tensor(out=A16.rearrange("p (t r) -> p t r", t=4), in0=dtt[db].rearrange("p (t r) -> p t r", t=4),
                                                             in1=a_neg[:, hg * 4:hg * 4 + 4].unsqueeze(1).to_broadcast([128, 4, 4]), op=ALU.mult),
                         r=[dtB[db], miscB], w=[smvB])
                    ps, pb = ps_m()
                    T.op("tensor", lambda e: e.matmul(ps[:, 0:16], tri, A16, start=True, stop=True), r=[smvB, cstB], w=[pb], sig=False)
                    T.op("tensor", lambda e: e.matmul(ps[:, 16:32], ones32, A16, start=True, stop=True), r=[smvB, cstB], w=[pb])
                    T.op("vector", lambda e: e.tensor_copy(out=smv[:, 16:48], in_=ps[:, 0:32]), r=[pb], w=[smvB])
                    T.op("scalar", lambda e: e.activation(out=etot16, in_=tot16, func=AF.Exp), w=[smvB])
                    T.op("vector", lambda e: e.tensor_tensor(out=dec16, in0=tot16, in1=Acs16, op=ALU.subtract), w=[smvB])
                    T.op("scalar", lambda e: e.activation(out=dec16, in_=dec16, func=AF.Exp), w=[smvB])
                    if own_chunk:
                        T.op("scalar", lambda e: e.activation(out=eAcs16, in_=Acs16, func=AF.Exp), w=[smvB])
                    T.op("vector", lambda e: e.tensor_tensor(out=w16_, in0=dtt[db], in1=dec16, op=ALU.mult), r=[dtB[db]], w=[smvB])
                    T.op("vector", lambda e: e.tensor_tensor(out=w16_.rearrange("p (t r) -> p t r", t=4), in0=w16_.rearrange("p (t r) -> p t r", t=4),
                                                             in1=vtile[:, tc * 4:(tc + 1) * 4].unsqueeze(2).to_broadcast([128, 4, 4]), op=ALU.mult),
                         r=[smB], w=[smvB])
                    T.op("vector", lambda e: e.tensor_tensor(out=Xd.rearrange("p (n c) -> p n c", n=16), in0=xh16[db].rearrange("p (n c) -> p n c", n=16),
                                                             in1=b3(w16_, 16), op=ALU.mult),
                         r=[xhB[db], smvB], w=[XdB])
                steps.append(S)

                def Dst():
                    banks = [ps_s(), ps_s()]
                    for tt in range(4):
                        psd, pbd = banks[tt // 2]
                        c = (tt % 2) * 256
                        T.op("tensor", lambda e, tt=tt, psd=psd, c=c: e.matmul(psd[:, c:c + 256], Btok[db][:, tt * 128:(tt + 1) * 128], Xd[:, tt * 256:(tt + 1) * 256], start=True, stop=True),
                             r=[BtokB[db], XdB], w=[pbd], sig=(tt % 2 == 1))
                    for tt in range(4):
                        t = tc * 4 + tt
                        psd, pbd = banks[tt // 2]
                        c = (tt % 2) * 256
                        if t >= OWN0:
                            T.op("scalar", lambda e, tt=tt: e.activation(out=S16all[:, tt * 256:(tt + 1) * 256], in_=S32, func=AF.Copy), r=[SB_], w=[S16B])
                        T.op("vector", lambda e, tt=tt: e.tensor_tensor(out=S32.rearrange("p (r c) -> p r c", r=4), in0=S32.rearrange("p (r c) -> p r c", r=4),
                                                                        in1=b3(etot16[:, tt * 4:tt * 4 + 4], 4), op=ALU.mult), r=[smvB], w=[SB_])
                        T.op("vector", lambda e, psd=psd, c=c: e.tensor_tensor(out=S32, in0=S32, in1=psd[:, c:c + 256], op=ALU.add), r=[pbd], w=[SB_])
                steps.append(Dst)

                def O(tt):
                    t = tc * 4 + tt
                    qi = t - OWN0
                    so_ = tt % 2
                    lhsD4 = eD4 = lhsD4s[so_]
                    lhsDB = eDB = LDBs[so_]
                    MT4, MTB = MT4s[so_], MTBs[so_]
                    yoff, yoffB, Gm, GmB = yoffs[so_], yoffBs[so_], Gms[so_], GmBs[so_]
                    pso, pbo = ps_s()
                    T.op("tensor", lambda e: e.matmul(pso[:, 0:256], CT16[db][:, tt * 128:(tt + 1) * 128], S16all[:, tt * 256:(tt + 1) * 256], start=True, stop=True),
                         r=[CTB[db], S16B], w=[pbo], sig=False)
                    T.op("tensor", lambda e: e.matmul(pso[:, 256:384], BT16[db][:, tt * 128:(tt + 1) * 128], CT16[db][:, tt * 128:(tt + 1) * 128], start=True, stop=True),
                         r=[BTB[db], CTB[db]], w=[pbo])
                    T.op("vector", lambda e: e.tensor_tensor(out=yoff.rearrange("p (r c) -> p r c", r=4), in0=pso[:, 0:256].rearrange("p (r c) -> p r c", r=4),
                                                             in1=b3(eAcs16[:, tt * 4:tt * 4 + 4], 4), op=ALU.mult), r=[pbo, smvB], w=[yoffB])
                    T.op("vector", lambda e: e.tensor_tensor(out=Gm, in0=pso[:, 256:384], in1=tri, op=ALU.mult), r=[pbo, cstB], w=[GmB])
                    T.op("vector", lambda e: e.tensor_tensor(out=lhsD4.rearrange("p (r c) -> p r c", r=4), in0=umat.unsqueeze(1).to_broadcast([128, 4, 128]),
                                                             in1=A16[:, tt * 4:tt * 4 + 4].unsqueeze(2).to_broadcast([128, 4, 128]), op=ALU.mult),
                         r=[smvB, cstB], w=[lhsDB])
                    psD, pbD = (PS[4 + tt % 2], PB[4 + tt % 2])
                    for r_ in range(4):
                        T.op("tensor", lambda e, r_=r_: e.matmul(psD[:, r_ * 128:(r_ + 1) * 128], lhsD4[:, r_ * 128:(r_ + 1) * 128], tri, start=True, stop=True),
                             r=[lhsDB, cstB], w=[pbD], sig=(r_ == 3))
                    T.op("scalar", lambda e: e.activation(out=eD4, in_=psD[:, :], func=AF.Exp), r=[pbD], w=[eDB])
                    T.op("vector", lambda e: e.tensor_tensor(out=eD4.rearrange("p (r c) -> p r c", r=4), in0=eD4.rearrange("p (r c) -> p r c", r=4),
                                                             in1=Gm.unsqueeze(1).to_broadcast([128, 4, 128]), op=ALU.mult), r=[GmB], w=[eDB])
                    T.op("vector", lambda e: e.tensor_tensor(out=MT4.rearrange("p (r c) -> p r c", r=4), in0=eD4.rearrange("p (r c) -> p r c", r=4),
                                                             in1=dtt[db][:, tt * 4:tt * 4 + 4].unsqueeze(2).to_broadcast([128, 4, 128]), op=ALU.mult),
                         r=[eDB, dtB[db]], w=[MTB])
                    psy, pby = (PS[6], PB[6])
                    for r_ in range(4):
                        T.op("tensor", lambda e, r_=r_: e.matmul(psy[:, r_ * 64:(r_ + 1) * 64], MT4[:, r_ * 128:(r_ + 1) * 128], xh16[db][:, tt * 256 + r_ * 64:tt * 256 + (r_ + 1) * 64], start=True, stop=True),
                             r=[MTB, xhB[db]], w=[pby], sig=(r_ == 3))
                    yo = yg[qi % 2]
                    yoB = ygB[qi % 2]
                    T.op("vector", lambda e: e.tensor_tensor(out=yo, in0=psy[:, 0:256], in1=yoff, op=ALU.add), r=[pby, yoffB], w=[yoB])
                    T.op("vector", lambda e: e.tensor_tensor(out=yoff.rearrange("p (r c) -> p r c", r=4), in0=xh16[db][:, tt * 256:(tt + 1) * 256].rearrange("p (r c) -> p r c", r=4),
                                                             in1=b3(dsk[:, hg * 4:hg * 4 + 4], 4), op=ALU.mult),
                         r=[xhB[db], smB], w=[yoffB])
                    T.op("vector", lambda e: e.tensor_tensor(out=yo, in0=yo, in1=yoff, op=ALU.add), r=[yoffB], w=[yoB])
                    T.op("vector", lambda e: e.tensor_tensor(out=yo, in0=yo, in1=zs[db][:, tt * 256:(tt + 1) * 256], op=ALU.mult), r=[zsB[db]], w=[yoB])
                    T.op("scalar", lambda e: e.activation(out=junk[:, 0:256], in_=yo, func=AF.Square, accum_out=ssq_s[:, qi * 4 + hg:qi * 4 + hg + 1]),
                         r=[yoB], w=[junkB, ssqB])
                    T.dma("scalar", ymix[qi * 128:(qi + 1) * 128, 1024 + hg * 256:1024 + (hg + 1) * 256], yo, yoB, r=[yoB])

                for tt in range(4):
                    if tc * 4 + tt >= OWN0:
                        steps.append(lambda tt=tt: O(tt))
                return steps

            for st_ in early(0):
                st_()
            for tc in range(8):
                L = late(tc)
                E = early(tc + 1) if tc + 1 < 8 else []
                for i in range(max(len(L), len(E))):
                    if i < len(E):
                        E[i]()
                    if i < len(L):
                        L[i]()
        rr["n"] = 3
        phase_done(3)

        A.off = base0
        regX = A.off
        A.off = regX + NCT * 512
        regH = A.off
        h2T = A.bf16(16 * NOWN * 128)
        h2Tv = h2T.rearrange("p (k t) -> p k t", k=16)
        h2TB = Buf("h2T")
        regR = A.off
        A.off = regX
        wo16 = A.bf16(16 * 2048)
        wo16v = wo16.rearrange("p (k c) -> p k c", k=16)
        woB = Buf("wo16")
        wst = A.f32(2048)
        wstB = Buf("wst3")
        Yt = [A.f32(2048), A.f32(2048)]
        YtB = [Buf("Yt0"), Buf("Yt1")]
        assert A.off <= regH, A.off
        A.off = regR
        mx16 = A.bf16(2048)
        mxB = Buf("mx16")
        mxT = A.bf16(2048)
        mxTv = mxT.rearrange("p (k t) -> p k t", k=16)
        mxTB = Buf("mxT")
        xr = A.f32(2048)
        xrB = Buf("xr")
        gate_m = A.f32(2048)
        gs_f = A.f32(2048)
        sh_f = A.f32(2048)
        gfB = Buf("gsf")
        gmB_ = Buf("gate_m")
        h2_16 = A.bf16(2048)
        h2B = Buf("h2_16")
        rs = A.f32(16)
        rsB = Buf("rs")
        junkC = A.bf16(2048)
        w_out_v = w_out.rearrange("(k p) n -> p k n", p=128)
        for kc in range(16):
            T.dma("sync", wst, w_out_v[:, kc, :], wstB, w=[wstB])
            T.op("vector" if kc % 2 else "scalar",
                 (lambda e, kc=kc: e.tensor_scalar(out=wo16v[:, kc, :], in0=wst, scalar1=gcat[:, kc:kc + 1], scalar2=None, op0=ALU.mult)) if kc % 2 else
                 (lambda e, kc=kc: e.activation(out=wo16v[:, kc, :], in_=wst, func=AF.Copy, scale=gcat[:, kc:kc + 1])),
                 r=[wstB, smB], w=[woB])
        bcast_row(gate_m, gmB_, modv[2:3, :])
        load_gs(gs_f, gfB, sh_f, 1, 3, 4, xr, xrB)
        for qi in range(NOWN):
            t = OWN0 + qi
            Y = Yt[qi % 2]
            YB = YtB[qi % 2]
            T.dma("sync", Y, ymix[qi * 128:(qi + 1) * 128, :], YB, w=[YB])
            T.dma("sync", xr, xl_t[t], xrB, w=[xrB])
            T.op("vector", lambda e, qi=qi: e.tensor_reduce(out=rs[:, 0:1], in_=ssq_a[:, qi * 8:(qi + 1) * 8], axis=AX.X, op=ALU.add), r=[ssqB], w=[rsB])
            T.op("vector", lambda e, qi=qi: e.tensor_reduce(out=rs[:, 1:3], in_=ssq_s[:, qi * 4:(qi + 1) * 4].rearrange("p (g c) -> p g c", g=2), axis=AX.X, op=ALU.add), r=[ssqB], w=[rsB])
            T.op("vector", lambda e: e.tensor_scalar(out=rs[:, 0:1], in0=rs[:, 0:1], scalar1=1.0 / 1024, scalar2=EPS, op0=ALU.mult, op1=ALU.add), w=[rsB])
            T.op("vector", lambda e: e.tensor_scalar(out=rs[:, 1:3], in0=rs[:, 1:3], scalar1=1.0 / 512, scalar2=EPS, op0=ALU.mult, op1=ALU.add), w=[rsB])
            T.op("scalar", lambda e: e.activation(out=rs[:, 0:3], in_=rs[:, 0:3], func=AF.Sqrt), w=[rsB])
            T.op("vector", lambda e: e.reciprocal(out=rs[:, 0:3], in_=rs[:, 0:3]), w=[rsB])
            T.op("vector", lambda e, Y=Y: e.tensor_scalar(out=mx16[:, 0:1024], in0=Y[:, 0:1024], scalar1=rs[:, 0:1], scalar2=None, op0=ALU.mult), r=[YB, rsB], w=[mxB])
            T.op("vector", lambda e, Y=Y: e.tensor_scalar(out=mx16[:, 1024:1536], in0=Y[:, 1024:1536], scalar1=rs[:, 1:2], scalar2=None, op0=ALU.mult), r=[YB, rsB], w=[mxB])
            T.op("vector", lambda e, Y=Y: e.tensor_scalar(out=mx16[:, 1536:2048], in0=Y[:, 1536:2048], scalar1=rs[:, 2:3], scalar2=None, op0=ALU.mult), r=[YB, rsB], w=[mxB])
            transpose_to(mx16, mxB, mxTv, mxTB, 0)
            for dc in range(4):
                ps, pb = ACCP[dc]
                for kc in range(16):
                    T.op("tensor", lambda e, ps=ps, kc=kc, dc=dc: e.matmul(ps[:, :], mxTv[:, kc, :], wo16v[:, kc, dc * 512:(dc + 1) * 512], start=(kc == 0), stop=(kc == 15)),
                         r=[mxTB, woB], w=[pb], sig=(kc == 15))
                T.op("vector", lambda e, ps=ps, dc=dc, Y=Y: e.tensor_tensor(out=Y[:, dc * 512:(dc + 1) * 512], in0=ps[:, :], in1=gate_m[:, dc * 512:(dc + 1) * 512], op=ALU.mult),
                     r=[pb, gmB_, mxB], w=[YB])
            T.op("vector", lambda e, Y=Y: e.tensor_tensor(out=xr, in0=xr, in1=Y, op=ALU.add), r=[YB], w=[xrB])
            if qi >= 1:
                T.dma("scalar", xmid[(qi - 1) * 128:qi * 128, :], xr, xrB, r=[xrB])
            norm_tile(xr, xrB, gs_f, sh_f, gfB, vtile[:, t:t + 1], h2_16, h2B, rs[:, 8:16], junkC)
            transpose_to(h2_16, h2B, h2Tv, h2TB, qi * 128)
        phase_done(4)

        A.off = regX
        aT = A.bf16(NCT * 1024)
        aTv = aT.rearrange("p (c t) -> p c t", c=NCT)
        aTB = Buf("aT")
        assert A.off == regH
        A.off = regR
        wu32 = [[A.f32(2048), A.f32(2048)], [A.f32(2048), A.f32(2048)]]
        wu32B = [[Buf("wu32_%d%d" % (a, b)) for b in range(2)] for a in range(2)]
        wu16 = [[A.bf16(2048), A.bf16(2048)], [A.bf16(2048), A.bf16(2048)]]
        wu16B = [[Buf("wu16_%d%d" % (a, b)) for b in range(2)] for a in range(2)]
        ub2 = [A.f32(1026), A.f32(1026)]
        ub2B = [Buf("ub2g"), Buf("ub2v")]
        cv2 = [A.f32(1024), A.f32(1024)]
        cv2B = [Buf("cv2g"), Buf("cv2v")]
        w_up_v = w_up.rearrange("(k p) n -> p k n", p=128)

        def load_up(ct):
            s = ct % 2
            for gv in range(2):
                col0 = gv * FFN + ct * 128
                T.dma("sync", wu32[s][gv].rearrange("p (k c) -> p k c", k=16), w_up_v[:, :, col0:col0 + 128], wu32B[s][gv], w=[wu32B[s][gv]])

        load_up(0)
        for ct in range(NCT):
            s = ct % 2
            if ct + 1 < NCT:
                load_up(ct + 1)
            for gv in range(2):
                if gv == 0:
                    T.op("scalar", lambda e, s=s, gv=gv: e.activation(out=wu16[s][gv], in_=wu32[s][gv], func=AF.Copy), r=[wu32B[s][gv]], w=[wu16B[s][gv]])
                else:
                    T.op("gpsimd", lambda e, s=s, gv=gv: e.tensor_copy(out=wu16[s][gv], in_=wu32[s][gv]), r=[wu32B[s][gv]], w=[wu16B[s][gv]])
            for gv in range(2):
                wv_ = wu16[s][gv].rearrange("p (k c) -> p k c", k=16)
                tile_i = gv * NCT + ct
                eng = "vector"
                ps, pb = ps_s()
                for kc in range(16):
                    T.op("tensor", lambda e, ps=ps, kc=kc, wv_=wv_: e.matmul(ps[:, 0:128], wv_[:, kc, :], h2Tv[:, kc, 0:128], start=(kc == 0), stop=(kc == 15)),
                         r=[wu16B[s][gv], h2TB], w=[pb], sig=(kc == 15))
                T.op("scalar", lambda e, ps=ps, gv=gv: e.activation(out=ub2[gv][:, 0:2], in_=ps[:, 126:128], func=AF.Copy), r=[pb], w=[ub2B[gv]])
                for half in range(2):
                    ps, pb = ps_s()
                    for kc in range(16):
                        T.op("tensor", lambda e, ps=ps, kc=kc, wv_=wv_, half=half: e.matmul(ps[:, :], wv_[:, kc, :], h2Tv[:, kc, 128 + half * 512:128 + (half + 1) * 512], start=(kc == 0), stop=(kc == 15)),
                             r=[wu16B[s][gv], h2TB], w=[pb], sig=(kc == 15))
                    T.op("scalar", lambda e, ps=ps, gv=gv, half=half: e.activation(out=ub2[gv][:, 2 + half * 512:2 + (half + 1) * 512], in_=ps[:, :], func=AF.Copy), r=[pb], w=[ub2B[gv]])
                T.op(eng, lambda e, gv=gv, tile_i=tile_i: e.tensor_scalar(out=cv2[gv], in0=ub2[gv][:, 2:1026], scalar1=cwf[:, tile_i * 3 + 2:tile_i * 3 + 3], scalar2=cbf[:, tile_i:tile_i + 1], op0=ALU.mult, op1=ALU.add),
                     r=[ub2B[gv], smB], w=[cv2B[gv]])
                for jj in range(1, 3):
                    T.op(eng, lambda e, gv=gv, tile_i=tile_i, jj=jj: e.scalar_tensor_tensor(out=cv2[gv], in0=ub2[gv][:, 2 - jj:1026 - jj], scalar=cwf[:, tile_i * 3 + 2 - jj:tile_i * 3 + 3 - jj], in1=cv2[gv], op0=ALU.mult, op1=ALU.add),
                         r=[ub2B[gv], smB], w=[cv2B[gv]])
            T.op("scalar", lambda e: e.activation(out=cv2[0], in_=cv2[0], func=AF.Silu), w=[cv2B[0]])
            T.op("vector", lambda e, ct=ct: e.tensor_tensor(out=aTv[:, ct, :], in0=cv2[0], in1=cv2[1], op=ALU.mult), r=[cv2B[0], cv2B[1]], w=[aTB])
        phase_done(5)

        A.off = regH
        wd32 = [A.f32(NCT * 128), A.f32(NCT * 128)]
        wd32B = [Buf("wd32a"), Buf("wd32b")]
        wd16 = [A.bf16(NCT * 128), A.bf16(NCT * 128)]
        wd16B = [Buf("wd16a"), Buf("wd16b")]
        ffT = [A.f32(512), A.f32(512)]
        ffTB = [Buf("ffT0"), Buf("ffT1")]
        ftok = [A.f32(512), A.f32(512)]
        ftokB = [Buf("ftok0"), Buf("ftok1")]
        w_down_v = w_down.rearrange("(c p) n -> p c n", p=128)
        ffsc_v = ffsc.rearrange("(t p) d -> p t d", p=128)

        def load_wd(dt_):
            s_ = dt_ % 2
            T.dma("sync", wd32[s_].rearrange("p (c n) -> p c n", c=NCT), w_down_v[:, :, dt_ * 128:(dt_ + 1) * 128], wd32B[s_], w=[wd32B[s_]])

        load_wd(0)
        it = 0
        for dt_ in range(16):
            s_ = dt_ % 2
            if dt_ + 1 < 16:
                load_wd(dt_ + 1)
            if s_ == 0:
                T.op("scalar", lambda e, s_=s_: e.activation(out=wd16[s_], in_=wd32[s_], func=AF.Copy), r=[wd32B[s_]], w=[wd16B[s_]])
            else:
                T.op("gpsimd", lambda e, s_=s_: e.tensor_copy(out=wd16[s_], in_=wd32[s_]), r=[wd32B[s_]], w=[wd16B[s_]])
            wdv = wd16[s_].rearrange("p (c n) -> p c n", c=NCT)
            for half in range(2):
                fs = it % 2
                it += 1
                ps, pb = ps_s()
                for ct in range(NCT):
                    T.op("tensor", lambda e, ps=ps, ct=ct, wdv=wdv, half=half: e.matmul(ps[:, :], wdv[:, ct, :], aTv[:, ct, half * 512:(half + 1) * 512], start=(ct == 0), stop=(ct == NCT - 1)),
                         r=[wd16B[s_], aTB], w=[pb], sig=(ct == NCT - 1))
                if fs == 0:
                    T.op("scalar", lambda e, ps=ps, fs=fs: e.activation(out=ffT[fs], in_=ps[:, :], func=AF.Copy), r=[pb], w=[ffTB[fs]])
                else:
                    T.op("vector", lambda e, ps=ps, fs=fs: e.tensor_copy(out=ffT[fs], in_=ps[:, :]), r=[pb], w=[ffTB[fs]])
                ps2, pb2 = ACCP[it % 4]
                for tt in range(4):
                    T.op("tensor", lambda e, ps2=ps2, tt=tt, fs=fs: e.transpose(ps2[:, tt * 128:(tt + 1) * 128], ffT[fs][:, tt * 128:(tt + 1) * 128], ident32),
                         r=[ffTB[fs], cstB], w=[pb2], sig=(tt == 3))
                T.op("vector", lambda e, ps2=ps2, fs=fs: e.tensor_copy(out=ftok[fs], in_=ps2[:, :]), r=[pb2], w=[ftokB[fs]])
                T.dma("scalar", ffsc_v[:, half * 4:(half + 1) * 4, dt_ * 128:(dt_ + 1) * 128], ftok[fs].rearrange("p (t d) -> p t d", t=4), ftokB[fs], r=[ftokB[fs]])
        T.barrier()

        A.off = regH
        fft_ = [A.f32(2048), A.f32(2048)]
        fftB = [Buf("fft0"), Buf("fft1")]
        xm_ = [A.f32(2048), A.f32(2048)]
        xmB_ = [Buf("xm0"), Buf("xm1")]
        gate_f = A.f32(2048)
        fng = A.f32(2048)
        gffB = Buf("gate_f")
        st2 = A.f32(16)
        st2B = Buf("st2")
        bcast_row(gate_f, gffB, modv[5:6, :])
        bcast_row(fng, gffB, vecs[2:3, :])

        def load_fin(tok):
            s_ = tok % 2
            T.dma("sync", fft_[s_], ffsc[tok * 128:(tok + 1) * 128, :], fftB[s_], w=[fftB[s_]])
            T.dma("sync", xm_[s_], xmid[tok * 128:(tok + 1) * 128, :], xmB_[s_], w=[xmB_[s_]])

        load_fin(0)
        for tok in range(8):
            s_ = tok % 2
            if tok + 1 < 8:
                load_fin(tok + 1)
            fft, xm, fB, xB_ = fft_[s_], xm_[s_], fftB[s_], xmB_[s_]
            T.op("gpsimd", lambda e, fft=fft: e.tensor_tensor(out=fft, in0=fft, in1=gate_f, op=ALU.mult), r=[gffB], w=[fB])
            T.op("vector", lambda e, fft=fft, xm=xm: e.tensor_tensor(out=xm, in0=xm, in1=fft, op=ALU.add), r=[fB], w=[xB_])
            T.op("vector", lambda e: e.memset(st2[:, 0:1], 0.0), w=[st2B])
            T.op("scalar", lambda e, fft=fft, xm=xm: e.activation(out=fft, in_=xm, func=AF.Square, accum_out=st2[:, 0:1]), r=[xB_], w=[fB, st2B])
            T.op("vector", lambda e: e.tensor_scalar(out=st2[:, 1:2], in0=st2[:, 0:1], scalar1=1.0 / D, scalar2=EPS, op0=ALU.mult, op1=ALU.add), w=[st2B])
            T.op("scalar", lambda e: e.activation(out=st2[:, 1:2], in_=st2[:, 1:2], func=AF.Sqrt), w=[st2B])
            T.op("vector", lambda e: e.reciprocal(out=st2[:, 1:2], in_=st2[:, 1:2]), w=[st2B])
            T.op("vector", lambda e, xm=xm: e.scalar_tensor_tensor(out=xm, in0=xm, scalar=st2[:, 1:2], in1=fng, op0=ALU.mult, op1=ALU.mult), r=[gffB, st2B], w=[xB_])
            T.dma("scalar", y_out[tok * 128:(tok + 1) * 128, :], xm, xB_, r=[xB_])
        phase_done(6)


    try:
        body()
    except _Stop:
        print('STOPPED after phase', KSTOP)

    engs = {"sync": nc.sync, "scalar": nc.scalar, "vector": nc.vector, "gpsimd": nc.gpsimd, "tensor": nc.tensor}
    with nc.Block() as block:
        @block.sync
        def _(e):
            for f in T.streams["sync"]:
                f(e)

        @block.scalar
        def _(e):
            for f in T.streams["scalar"]:
                f(e)

        @block.vector
        def _(e):
            for f in T.streams["vector"]:
                f(e)

        @block.gpsimd
        def _(e):
            for f in T.streams["gpsimd"]:
                f(e)

        @block.tensor
        def _(e):
            for f in T.streams["tensor"]:
                f(e)
    print("instr counts", {k: len(v) for k, v in T.streams.items()}, "sem counts", T.ecnt, "nsem", T.nsem, "arena hi", A.hi)
    return nc


def rel_bucket_np(dist):
    n = np.maximum(dist, 0)
    max_exact = 16
    nf = np.maximum(n, max_exact).astype(np.float32)
    large = max_exact + (np.log(nf / max_exact) / np.float32(np.log(128 / max_exact)) * (32 - max_exact)).astype(np.int32)
    large = np.minimum(large, 31)
    return np.where(n < max_exact, n, large)


_NC_CACHE = {}


def kernel(x, c, w_ada, b_ada, norm_mix_g, w_in, rel_bias, attn_norm_g, conv_ssm_w, conv_ssm_b,
           dt_bias, a_log, d_skip, ssm_norm_g, w_out, norm_ffn_g, w_up, conv_ffn_w, conv_ffn_b,
           w_down, final_norm_g):
    if "nc" not in _NC_CACHE:
        _NC_CACHE["nc"] = build_nc()
    nc = _NC_CACHE["nc"]
    in_maps = prep_inputs(x, c, w_ada, b_ada, norm_mix_g, w_in, rel_bias, attn_norm_g, conv_ssm_w, conv_ssm_b,
                          dt_bias, a_log, d_skip, ssm_norm_g, w_out, norm_ffn_g, w_up, conv_ffn_w, conv_ffn_b,
                          w_down, final_norm_g)
    res = run_bass_kernel_spmd(nc, in_maps, core_ids=list(range(8)))
    out = np.zeros((2, 4096, D), np.float32)
    for core in range(8):
        b, j = core // 4, core % 4
        out[b, j * 1024:(j + 1) * 1024] = res.results[core]["y"]
    return out


def prep_inputs(x, c, w_ada, b_ada, norm_mix_g, w_in, rel_bias, attn_norm_g, conv_ssm_w, conv_ssm_b,
                dt_bias, a_log, d_skip, ssm_norm_g, w_out, norm_ffn_g, w_up, conv_ffn_w, conv_ffn_b,
                w_down, final_norm_g):
    f = np.float32
    x = np.asarray(x, f)
    consts = np.zeros((128, 512), f)
    k = np.arange(128)
    consts[:, 0:128] = np.eye(128, dtype=f)
    consts[:, 128:256] = (k[:, None] <= k[None, :]).astype(f)
    consts[:, 256:384] = (k[:, None] > k[None, :]).astype(f)
    consts[:, 384:512] = 1.0
    eall = np.zeros((16, 2048), f)
    for n in range(16):
        eall[n, n * 128:(n + 1) * 128] = 1.0
    kk = np.arange(128)[:, None]
    qq = np.arange(512)[None, :]
    btiles = np.zeros((8, 5, 128, 512), f)
    rb = np.asarray(rel_bias, f)
    for oi, off in enumerate(OFFS):
        dist = off + qq - kk
        bk = rel_bucket_np(dist)
        for h in range(8):
            btiles[h, oi] = np.where(dist >= 0, rb[bk, h], f(NEG))
    sm_common = np.zeros((128, 560), f)
    sm_common[:, 64:80] = np.asarray(dt_bias, f)[0][None, :]
    sm_common[:, 80:96] = np.asarray(a_log, f)[0][None, :]
    sm_common[:, 96:112] = np.asarray(d_skip, f)[0][None, :]
    sm_common[:, 112:120] = rb[31][None, :]
    gc = np.concatenate([np.asarray(attn_norm_g, f)[0], np.asarray(ssm_norm_g, f)[0]])
    sm_common[:, 120:136] = gc.reshape(16, 128).T
    cw = np.asarray(conv_ssm_w, f)[0]
    sm_common[:, 136:184] = cw.reshape(4, 12, 128).transpose(2, 1, 0).reshape(128, 48)
    sm_common[:, 184:196] = np.asarray(conv_ssm_b, f)[0].reshape(12, 128).T
    cf = np.asarray(conv_ffn_w, f)[0]
    sm_common[:, 196:460] = cf.reshape(3, 88, 128).transpose(2, 1, 0).reshape(128, 264)
    sm_common[:, 460:548] = np.asarray(conv_ffn_b, f)[0].reshape(88, 128).T
    vecs = np.stack([np.asarray(norm_mix_g, f)[0], np.asarray(norm_ffn_g, f)[0], np.asarray(final_norm_g, f)])
    shared = {
        "consts": consts, "eall": eall, "w_ada": np.ascontiguousarray(np.asarray(w_ada, f)[0]),
        "b_ada": np.ascontiguousarray(np.asarray(b_ada, f)), "vecs": vecs,
        "w_in": np.ascontiguousarray(np.asarray(w_in, f)[0]), "btiles": btiles,
        "w_out": np.ascontiguousarray(np.asarray(w_out, f)[0]), "w_up": np.ascontiguousarray(np.asarray(w_up, f)[0]),
        "w_down": np.ascontiguousarray(np.asarray(w_down, f)[0]),
    }
    in_maps = []
    for core in range(8):
        b, j = core // 4, core % 4
        npad = (3 - j) * 1024
        xl = np.zeros((4096, D), f)
        xl[npad:] = x[b, :(j + 1) * 1024]
        sm = sm_common.copy()
        sm[:, 0:32] = (np.arange(32) * 128 >= npad).astype(f)[None, :]
        sm[:, 32:48] = np.where(np.arange(16) * 256 >= npad, f(0), f(-1e30))[None, :]
        sm[:, 48:64] = np.asarray(c, f)[b].reshape(128, 16)
        d = dict(shared)
        d["xl"] = xl
        d["smalls"] = sm
        in_maps.append(d)
    return in_maps
```
